# Optimizing a Trainium2 kernel written in Bass

```python
import math
import jax, jax.numpy as jnp
from jax import lax
import numpy as np

D_MODEL = 1024
BATCH = 8
SEQ = 2048
DEPTH = 2

N_GROUPS = 4
GROUP_WIDTH = D_MODEL // N_GROUPS
HEAD_DIM = 64
GROUP_HEADS = GROUP_WIDTH // HEAD_DIM
CONV_CH = GROUP_WIDTH
CONV_WIDTH = 3
NSA_HEADS = GROUP_HEADS
CMP_LEN = 32
CMP_STRIDE = 16
SLC_LEN = 64
N_SLC = 16
WINDOW = 512
CMP_HIDDEN = GROUP_WIDTH
MLA_HEADS = GROUP_HEADS
Q_LORA = 3 * GROUP_WIDTH // 4
KV_LORA = GROUP_WIDTH // 2
NOPE_DIM = HEAD_DIM
ROPE_DIM = HEAD_DIM // 2
V_DIM = HEAD_DIM
ROPE_THETA = 10000.0
SB_HEADS = GROUP_HEADS
N_BUCKETS = 32
MAX_DISTANCE = 128
D_FF = 4 * D_MODEL
Q_BLOCK = 128
EPS = 1e-6

A_SIZES = [CONV_CH] * 3
NSA_SIZES = [NSA_HEADS * HEAD_DIM] + [HEAD_DIM] * 6 + [3 * NSA_HEADS]
MLA_SIZES = [Q_LORA, KV_LORA, ROPE_DIM]
SB_SIZES = [SB_HEADS * HEAD_DIM] * 3
IN_SIZES = A_SIZES + NSA_SIZES + MLA_SIZES + SB_SIZES
IN_COLS = sum(IN_SIZES)

kernel_name = "hymba_style_conv_nsa_mla_stickbreaking_hybrid"

f32 = jnp.float32


def rms_norm(x, w):
    x32 = x.astype(f32)
    y = x32 * lax.rsqrt(jnp.mean(x32 * x32, axis=-1, keepdims=True) + EPS)
    return (y * w.astype(f32)).astype(x.dtype)


def masked_softmax(logits, mask):
    logits = jnp.where(mask, logits, -jnp.inf)
    m = jnp.max(logits, axis=-1, keepdims=True)
    m = jnp.where(jnp.isfinite(m), m, 0.0)
    p = jnp.exp(logits - m)
    s = jnp.sum(p, axis=-1, keepdims=True)
    return p / jnp.where(s > 0, s, 1.0)


def t5_bucket(dist):
    max_exact = N_BUCKETS // 2
    d = jnp.maximum(dist, 0)
    large = max_exact + (jnp.log(jnp.maximum(d, 1).astype(f32) / max_exact)
                         / math.log(MAX_DISTANCE / max_exact) * (N_BUCKETS - max_exact)).astype(jnp.int32)
    large = jnp.minimum(large, N_BUCKETS - 1)
    return jnp.where(d < max_exact, d, large)


def rope_tables(S):
    inv = 1.0 / (ROPE_THETA ** (jnp.arange(0, ROPE_DIM, 2, dtype=f32) / ROPE_DIM))
    ang = jnp.arange(S, dtype=f32)[:, None] * inv[None, :]
    return jnp.cos(ang), jnp.sin(ang)


def apply_rope(x, cos, sin):
    x32 = x.astype(f32)
    x1, x2 = jnp.split(x32, 2, axis=-1)
    c = cos[:, None, :]
    s = sin[:, None, :]
    return jnp.concatenate([x1 * c - x2 * s, x1 * s + x2 * c], axis=-1).astype(x.dtype)


def split_cols(h, sizes):
    return jnp.split(h, np.cumsum(sizes)[:-1].tolist(), axis=-1)


def short_conv_mixer(b_gate, c_gate, u, conv_w, conv_b):
    v = c_gate * u
    y = lax.conv_general_dilated(v, conv_w[:, None, :].astype(v.dtype), window_strides=(1,),
                                 padding=[(CONV_WIDTH - 1, 0)], dimension_numbers=('NWC', 'WIO', 'NWC'),
                                 feature_group_count=v.shape[-1])
    return b_gate * (y + conv_b)


def compress_tokens(kv, cmp_idx, pos, w1, w2):
    B = kv.shape[0]
    blocks = kv[:, cmp_idx] + pos
    hdn = jax.nn.silu(blocks.reshape(B, blocks.shape[1], -1) @ w1)
    return hdn @ w2


def cmp_slc_overlap(n_cmp, n_slc):
    starts = np.arange(n_cmp) * CMP_STRIDE
    ends = starts + CMP_LEN
    s0 = np.arange(n_slc) * SLC_LEN
    s1 = s0 + SLC_LEN
    ov = np.clip(np.minimum(ends[:, None], s1[None]) - np.maximum(starts[:, None], s0[None]), 0, None)
    return (ov / CMP_LEN).astype(np.float32)


def nsa_mixer(q, kc_raw, vc_raw, ks, vs, kw, vw, gate_logits, qn, kn, cmp_pos, cmp_w1, cmp_w2, rel_bias):
    B, S, H, Dh = q.shape
    scale = Dh ** -0.5
    q = rms_norm(q, qn)
    tpos = jnp.arange(S)
    n_cmp = (S - CMP_LEN) // CMP_STRIDE + 1
    cmp_idx = np.arange(n_cmp)[:, None] * CMP_STRIDE + np.arange(CMP_LEN)[None, :]
    kc = rms_norm(compress_tokens(kc_raw, cmp_idx, cmp_pos[0], cmp_w1[0], cmp_w2[0]), kn[0])
    vc = compress_tokens(vc_raw, cmp_idx, cmp_pos[1], cmp_w1[1], cmp_w2[1])
    cmp_end = jnp.arange(n_cmp) * CMP_STRIDE + CMP_LEN - 1
    dist_c = tpos[:, None] - cmp_end[None, :]
    bias_c = rel_bias[t5_bucket(dist_c)].transpose(2, 0, 1)
    logits_c = jnp.einsum('bshd,bnd->bhsn', q, kc).astype(f32) * scale + bias_c
    p_c = masked_softmax(logits_c, dist_c >= 0)
    o_cmp = jnp.einsum('bhsn,bnd->bshd', p_c.astype(vc.dtype), vc)
    n_slc = S // SLC_LEN
    n_sel = min(N_SLC, n_slc)
    score = jnp.einsum('bhsn,nj->bsj', p_c, jnp.asarray(cmp_slc_overlap(n_cmp, n_slc)))
    blk = jnp.arange(n_slc)[None, :]
    t_blk = (tpos // SLC_LEN)[:, None]
    valid = blk * SLC_LEN <= tpos[:, None]
    forced = (blk == 0) | (blk == t_blk) | (blk == t_blk - 1)
    score = jnp.where(forced, jnp.inf, jnp.where(valid, score, -jnp.inf))
    _, sel = lax.top_k(score, n_sel)
    nq = S // Q_BLOCK
    ks_blocks = rms_norm(ks, kn[1]).reshape(B, n_slc, SLC_LEN, Dh)
    vs_blocks = vs.reshape(B, n_slc, SLC_LEN, Dh)
    kw_pad = jnp.pad(rms_norm(kw, kn[2]), ((0, 0), (WINDOW, 0), (0, 0)))
    vw_pad = jnp.pad(vw, ((0, 0), (WINDOW, 0), (0, 0)))
    band = WINDOW + Q_BLOCK
    qi = jnp.arange(Q_BLOCK)
    ki = jnp.arange(band)
    dist_w = qi[:, None] + WINDOW - ki[None, :]
    bias_w = rel_bias[t5_bucket(dist_w)].transpose(2, 0, 1)
    in_window = (dist_w >= 0) & (dist_w < WINDOW)
    bidx = jnp.arange(B)[:, None, None]

    def block(args):
        q_blk, sel_blk, t0 = args
        t_q = t0 + qi
        kg = ks_blocks[bidx, sel_blk]
        vg = vs_blocks[bidx, sel_blk]
        kpos = (sel_blk[..., None] * SLC_LEN + jnp.arange(SLC_LEN)).reshape(B, Q_BLOCK, -1)
        dist_s = t_q[None, :, None] - kpos
        bias_s = rel_bias[t5_bucket(dist_s)].transpose(0, 3, 1, 2)
        logits_s = jnp.einsum('bqhd,bqnkd->bhqnk', q_blk, kg).reshape(B, H, Q_BLOCK, -1).astype(f32) * scale + bias_s
        p_s = masked_softmax(logits_s, (dist_s >= 0)[:, None])
        o_s = jnp.einsum('bhqnk,bqnkd->bqhd', p_s.reshape(B, H, Q_BLOCK, n_sel, SLC_LEN).astype(vg.dtype), vg)
        kb = lax.dynamic_slice_in_dim(kw_pad, t0, band, axis=1)
        vb = lax.dynamic_slice_in_dim(vw_pad, t0, band, axis=1)
        mask_w = in_window & (t0 - WINDOW + ki >= 0)[None, :]
        logits_w = jnp.einsum('bqhd,bkd->bhqk', q_blk, kb).astype(f32) * scale + bias_w
        p_w = masked_softmax(logits_w, mask_w)
        o_w = jnp.einsum('bhqk,bkd->bqhd', p_w.astype(vb.dtype), vb)
        return o_s, o_w

    q_blocks = q.reshape(B, nq, Q_BLOCK, H, Dh).swapaxes(0, 1)
    sel_blocks = sel.reshape(B, nq, Q_BLOCK, n_sel).swapaxes(0, 1)
    o_slc, o_win = lax.map(block, (q_blocks, sel_blocks, jnp.arange(nq) * Q_BLOCK))
    o_slc = o_slc.swapaxes(0, 1).reshape(B, S, H, Dh)
    o_win = o_win.swapaxes(0, 1).reshape(B, S, H, Dh)
    g = jax.nn.sigmoid(gate_logits.astype(f32)).reshape(B, S, H, 3).astype(q.dtype)
    out = g[..., 0:1] * o_cmp + g[..., 1:2] * o_slc + g[..., 2:3] * o_win
    return out.reshape(B, S, H * Dh)


def causal_block_attention(q, k, v, scale):
    B, S, H, _ = q.shape
    nq = S // Q_BLOCK
    kpos = jnp.arange(S)

    def one(args):
        q_blk, t0 = args
        logits = jnp.einsum('bqhd,bkhd->bhqk', q_blk, k).astype(f32) * scale
        mask = kpos[None, :] <= (t0 + jnp.arange(Q_BLOCK))[:, None]
        p = masked_softmax(logits, mask)
        return jnp.einsum('bhqk,bkhd->bqhd', p.astype(v.dtype), v)

    qb = q.reshape(B, nq, Q_BLOCK, H, -1).swapaxes(0, 1)
    out = lax.map(one, (qb, jnp.arange(nq) * Q_BLOCK))
    return out.swapaxes(0, 1).reshape(B, S, H, -1)


def mla_mixer(c_q, c_kv, k_rope, qa_norm, kv_norm, wq_b, wkv_b, qn, kn, cos, sin):
    B, S, _ = c_q.shape
    q = (rms_norm(c_q, qa_norm) @ wq_b).reshape(B, S, MLA_HEADS, NOPE_DIM + ROPE_DIM)
    q = jnp.concatenate([q[..., :NOPE_DIM], apply_rope(q[..., NOPE_DIM:], cos, sin)], axis=-1)
    kv = (rms_norm(c_kv, kv_norm) @ wkv_b).reshape(B, S, MLA_HEADS, NOPE_DIM + V_DIM)
    k_nope, v = kv[..., :NOPE_DIM], kv[..., NOPE_DIM:]
    k_r = apply_rope(k_rope[:, :, None, :], cos, sin)
    k = jnp.concatenate([k_nope, jnp.broadcast_to(k_r, (B, S, MLA_HEADS, ROPE_DIM))], axis=-1)
    q = rms_norm(q, qn)
    k = rms_norm(k, kn)
    o = causal_block_attention(q, k, v, (NOPE_DIM + ROPE_DIM) ** -0.5)
    return o.reshape(B, S, MLA_HEADS * V_DIM)


def stick_breaking_mixer(q, k, v):
    B, S, H, Dh = q.shape
    scale = Dh ** -0.5
    nq = S // Q_BLOCK
    kpos = jnp.arange(S)

    def one(args):
        q_blk, t0 = args
        z = jnp.einsum('bqhd,bkhd->bhqk', q_blk, k).astype(f32) * scale
        strict = kpos[None, :] < (t0 + jnp.arange(Q_BLOCK))[:, None]
        log_1m = jnp.where(strict, jax.nn.log_sigmoid(-z), 0.0)
        rem = lax.cumsum(log_1m, axis=3, reverse=True) - log_1m
        a = jnp.where(strict, jnp.exp(jax.nn.log_sigmoid(z) + rem), 0.0)
        return jnp.einsum('bhqk,bkhd->bqhd', a.astype(v.dtype), v)

    qb = q.reshape(B, nq, Q_BLOCK, H, Dh).swapaxes(0, 1)
    out = lax.map(one, (qb, jnp.arange(nq) * Q_BLOCK))
    return out.swapaxes(0, 1).reshape(B, S, H * Dh)


def setup_inputs(seed: int = 0) -> dict:
    key = jax.random.key(seed)
    ks = jax.random.split(key, 24)
    L = DEPTH
    nrm = lambda k, shape, s: jax.random.normal(k, shape, f32) * s
    gain = lambda k, shape: 1.0 + 0.02 * jax.random.normal(k, shape, f32)
    qk_dim = NOPE_DIM + ROPE_DIM
    return {
        "x": jax.random.normal(ks[0], (BATCH, SEQ, D_MODEL), f32),
        "rel_bias": nrm(ks[1], (N_BUCKETS, NSA_HEADS), 0.5),
        "norm1_w": gain(ks[2], (L, D_MODEL)),
        "w_in": nrm(ks[3], (L, D_MODEL, IN_COLS), D_MODEL ** -0.5),
        "conv_w": nrm(ks[4], (L, CONV_WIDTH, CONV_CH), CONV_WIDTH ** -0.5),
        "conv_b": nrm(ks[5], (L, CONV_CH), 0.02),
        "nsa_q_norm": gain(ks[6], (L, HEAD_DIM)),
        "nsa_k_norm": gain(ks[7], (L, 3, HEAD_DIM)),
        "cmp_pos": nrm(ks[8], (L, 2, CMP_LEN, HEAD_DIM), 0.1),
        "cmp_w1": nrm(ks[9], (L, 2, CMP_LEN * HEAD_DIM, CMP_HIDDEN), (CMP_LEN * HEAD_DIM) ** -0.5),
        "cmp_w2": nrm(ks[10], (L, 2, CMP_HIDDEN, HEAD_DIM), CMP_HIDDEN ** -0.5),
        "mla_q_a_norm": gain(ks[11], (L, Q_LORA)),
        "mla_kv_norm": gain(ks[12], (L, KV_LORA)),
        "mla_wq_b": nrm(ks[13], (L, Q_LORA, MLA_HEADS * qk_dim), Q_LORA ** -0.5),
        "mla_wkv_b": nrm(ks[14], (L, KV_LORA, MLA_HEADS * (NOPE_DIM + V_DIM)), KV_LORA ** -0.5),
        "mla_q_norm": gain(ks[15], (L, qk_dim)),
        "mla_k_norm": gain(ks[16], (L, qk_dim)),
        "out_norm_w": gain(ks[17], (L, D_MODEL)),
        "w_out": nrm(ks[18], (L, D_MODEL, D_MODEL), D_MODEL ** -0.5),
        "norm2_w": gain(ks[19], (L, D_MODEL)),
        "ffn_w1": nrm(ks[20], (L, D_MODEL, D_FF), D_MODEL ** -0.5),
        "ffn_w2": nrm(ks[21], (L, D_FF, D_MODEL), D_FF ** -0.5),
    }


def reference(x, rel_bias, norm1_w, w_in, conv_w, conv_b, nsa_q_norm, nsa_k_norm, cmp_pos, cmp_w1, cmp_w2,
              mla_q_a_norm, mla_kv_norm, mla_wq_b, mla_wkv_b, mla_q_norm, mla_k_norm, out_norm_w, w_out,
              norm2_w, ffn_w1, ffn_w2):
    B, S, D = x.shape
    cos, sin = rope_tables(S)
    for l in range(DEPTH):
        h = rms_norm(x, norm1_w[l])
        cols = split_cols(h @ w_in[l], IN_SIZES)
        a_b, a_c, a_u = cols[0:3]
        n_q, n_kc, n_vc, n_ks, n_vs, n_kw, n_vw, n_g = cols[3:11]
        m_cq, m_ckv, m_kr = cols[11:14]
        s_q, s_k, s_v = cols[14:17]
        y_a = short_conv_mixer(a_b, a_c, a_u, conv_w[l], conv_b[l])
        y_b = nsa_mixer(n_q.reshape(B, S, NSA_HEADS, HEAD_DIM), n_kc, n_vc, n_ks, n_vs, n_kw, n_vw, n_g,
                        nsa_q_norm[l], nsa_k_norm[l], cmp_pos[l], cmp_w1[l], cmp_w2[l], rel_bias)
        y_c = mla_mixer(m_cq, m_ckv, m_kr, mla_q_a_norm[l], mla_kv_norm[l], mla_wq_b[l], mla_wkv_b[l],
                        mla_q_norm[l], mla_k_norm[l], cos, sin)
        y_d = stick_breaking_mixer(s_q.reshape(B, S, SB_HEADS, HEAD_DIM), s_k.reshape(B, S, SB_HEADS, HEAD_DIM),
                                   s_v.reshape(B, S, SB_HEADS, HEAD_DIM))
        y = jnp.concatenate([y_a, y_b, y_c, y_d], axis=-1).reshape(B, S, N_GROUPS, GROUP_WIDTH)
        y = rms_norm(y, out_norm_w[l].reshape(N_GROUPS, GROUP_WIDTH)).reshape(B, S, D)
        x = x + y @ w_out[l]
        h2 = rms_norm(x, norm2_w[l])
        x = x + jnp.square(jax.nn.relu(h2 @ ffn_w1[l])) @ ffn_w2[l]
    return x
```

```python
import math
import os
from contextlib import ExitStack

import numpy as np
import concourse.bass as bass
import concourse.mybir as mybir
from concourse.bass_utils import run_bass_kernel_spmd

F32 = mybir.dt.float32
BF16 = mybir.dt.bfloat16
AF = mybir.ActivationFunctionType
ALU = mybir.AluOpType
AX = mybir.AxisListType

S = 2048
D = 1024
NT = 16
NL = 2
NEG = -30000.0
EPS = 1e-6
BIG = 1.0e9
IN_COLS = 2540
C_A, C_N, C_M, C_S = 0, 768, 1420, 1772

ENG_NAMES = ("pe", "act", "dve", "pool", "sp")
CUT = int(os.environ.get("NSA_CUT", "99"))


class Op:
    __slots__ = ("eng", "fn", "deps", "is_dma", "sem", "val", "idx", "signal", "snap", "waits", "cidx")

    def __init__(self, eng, fn, is_dma, sem):
        self.eng = eng
        self.fn = fn
        self.is_dma = is_dma
        self.sem = sem
        self.deps = set()
        self.val = 0
        self.signal = is_dma
        self.snap = None
        self.waits = []
        self.cidx = -1


class Prog:
    def __init__(self, nc):
        self.nc = nc
        self.ops = []
        self.last_w = {}
        self.readers = {}
        self.ncomp = {e: 0 for e in ENG_NAMES}

    def op(self, eng, fn, reads=(), writes=(), dma_sem=None, track=True):
        o = Op(eng, fn, dma_sem is not None, dma_sem if dma_sem is not None else eng)
        o.idx = len(self.ops)
        if not o.is_dma:
            o.cidx = self.ncomp[eng]
            self.ncomp[eng] += 1
        for r in reads:
            w = self.last_w.get(r)
            if w is not None:
                o.deps.add(w)
        for r in writes:
            w = self.last_w.get(r)
            if w is not None:
                o.deps.add(w)
            for x in self.readers.get(r, ()):
                o.deps.add(x)
        if track:
            for r in reads:
                self.readers.setdefault(r, []).append(o.idx)
            for r in writes:
                self.last_w[r] = o.idx
                self.readers[r] = []
        o.deps.discard(o.idx)
        self.ops.append(o)
        return o

    def _needs_wait(self, o, d):
        if d.is_dma or o.is_dma:
            return True
        if d.eng != o.eng:
            return True
        if o.eng == "pe":
            return False
        return (o.cidx - d.cidx) <= 1

    def finalize(self, sems):
        ops = self.ops
        for o in ops:
            for di in o.deps:
                d = ops[di]
                if self._needs_wait(o, d):
                    d.signal = True
        cnt = {}
        for o in ops:
            if o.signal:
                inc = 16 if o.is_dma else 1
                cnt[o.sem] = cnt.get(o.sem, 0) + inc
                o.val = cnt[o.sem]
        known = {e: {} for e in ENG_NAMES}
        for o in ops:
            k = known[o.eng]
            need = {}
            for di in o.deps:
                d = ops[di]
                if not self._needs_wait(o, d):
                    continue
                if k.get(d.sem, 0) >= d.val:
                    continue
                if need.get(d.sem, (0, None))[0] < d.val:
                    need[d.sem] = (d.val, d)
            for s, (v, d) in need.items():
                if k.get(s, 0) >= v:
                    continue
                o.waits.append((s, v))
                k[s] = v
                if d.snap:
                    for s2, v2 in d.snap.items():
                        if k.get(s2, 0) < v2:
                            k[s2] = v2
            if o.signal:
                o.snap = dict(k)
                if not o.is_dma:
                    o.snap[o.sem] = o.val
        per = {e: [] for e in ENG_NAMES}
        for o in ops:
            per[o.eng].append(o)

        def emit(engobj, lst):
            for o in lst:
                for s, v in o.waits:
                    engobj.wait_ge(sems[s], v)
                ins = o.fn(engobj)
                if o.signal:
                    assert ins is not None
                    ins.then_inc(sems[o.sem], 16 if o.is_dma else 1)

        with self.nc.Block() as block:
            @block.tensor
            def _(e):
                emit(e, per["pe"])

            @block.scalar
            def _(e):
                emit(e, per["act"])

            @block.vector
            def _(e):
                emit(e, per["dve"])

            @block.gpsimd
            def _(e):
                emit(e, per["pool"])

            @block.sync
            def _(e):
                emit(e, per["sp"])

    def sem_names(self):
        s = set(ENG_NAMES)
        for o in self.ops:
            s.add(o.sem)
        return sorted(s, key=str)


def _bucket(d):
    d = np.maximum(d, 0)
    df = np.maximum(d, 1).astype(np.float32)
    large = 16 + (np.log(df / np.float32(16)) / np.float32(math.log(8.0)) * np.float32(16)).astype(np.int32)
    large = np.minimum(large, 31)
    return np.where(d < 16, d, large)


def make_consts():
    c = {}
    c["c_ident"] = np.eye(128, dtype=np.float32)
    inv = (1.0 / (np.float32(10000.0) ** (np.arange(0, 32, 2, dtype=np.float32) / np.float32(32)))).astype(np.float32)
    ang = np.arange(S, dtype=np.float32)[:, None] * inv[None, :]
    cs = np.cos(ang).astype(np.float32).reshape(NT, 128, 16).transpose(1, 0, 2)
    sn = np.sin(ang).astype(np.float32).reshape(NT, 128, 16).transpose(1, 0, 2)
    c["c_cos"] = np.ascontiguousarray(cs)
    c["c_sin"] = np.ascontiguousarray(sn)
    oh = np.zeros((32, 1152), np.float32)
    negrow = np.zeros((1, 1152), np.float32)
    for i in range(256):
        u = i - 127
        if 0 <= u <= 127:
            oh[_bucket(np.array(u)), i] = 1.0
        else:
            negrow[0, i] = NEG
        if -127 <= u <= 127:
            oh[_bucket(np.array(128 + u)), 256 + i] = 1.0
        if u < 0:
            oh[31, 512 + i] = 1.0
        else:
            negrow[0, 512 + i] = NEG
    for i in range(384):
        dist = 240 - i
        if dist >= 0:
            oh[_bucket(np.array(dist)), 768 + i] = 1.0
        else:
            negrow[0, 768 + i] = NEG
    c["c_oh"] = oh
    c["c_negrow"] = negrow
    e = np.zeros((32, S), np.float32)
    e[np.arange(S) // 64, np.arange(S)] = 1.0
    c["c_e"] = e
    t = np.arange(S)
    blk = np.arange(32)[None, :]
    tb = (t // 64)[:, None]
    valid = blk * 64 <= t[:, None]
    forced = (blk == 0) | (blk == tb) | (blk == tb - 1)
    vm = (valid & ~forced).astype(np.float32)
    ad = np.where(forced, BIG, np.where(valid, 0.0, -BIG)).astype(np.float32)
    c["c_vm"] = np.ascontiguousarray(vm.reshape(NT, 128, 32).transpose(1, 0, 2))
    c["c_ad"] = np.ascontiguousarray(ad.reshape(NT, 128, 32).transpose(1, 0, 2))
    j = np.arange(128)
    c["c_tri"] = (j[:, None] >= j[None, :]).astype(np.float32)
    c["c_ones"] = np.ones((128, 128), np.float32)
    c["c_mcausal"] = np.where(j[None, :] >= j[:, None], 0.0, NEG).astype(np.float32)
    c["c_mstrict"] = (j[None, :] > j[:, None]).astype(np.float32)
    return c


CONST_SHAPES = {
    "c_ident": [128, 128], "c_cos": [128, NT, 16], "c_sin": [128, NT, 16], "c_oh": [32, 1152],
    "c_negrow": [1, 1152], "c_e": [32, S], "c_vm": [128, NT, 32], "c_ad": [128, NT, 32],
    "c_tri": [128, 128], "c_ones": [128, 128], "c_mcausal": [128, 128], "c_mstrict": [128, 128],
}

WEIGHT_SHAPES = {
    "rel_bias": [32, 4], "norm1_w": [NL, D], "w_in": [NL, D, IN_COLS], "conv_w": [NL, 3, 256], "conv_b": [NL, 256],
    "nsa_q_norm": [NL, 64], "nsa_k_norm": [NL, 3, 64], "cmp_pos": [NL, 2, 32, 64], "cmp_w1": [NL, 2, 2048, 256],
    "cmp_w2": [NL, 2, 256, 64], "mla_q_a_norm": [NL, 192], "mla_kv_norm": [NL, 128], "mla_wq_b": [NL, 192, 384],
    "mla_wkv_b": [NL, 128, 512], "mla_q_norm": [NL, 96], "mla_k_norm": [NL, 96], "out_norm_w": [NL, D],
    "w_out": [NL, D, D], "norm2_w": [NL, D], "ffn_w1": [NL, D, 4096], "ffn_w2": [NL, 4096, D],
}


class Builder:
    def __init__(self, stop_after=None, dbg=()):
        self.stop_after = stop_after
        self.dbg = set(dbg)
        self.nc = bass.Bass("TRN2", target_bir_lowering=False)
        self.P = Prog(self.nc)
        self.es = ExitStack()
        self.dram = {}
        self.dbg_outs = {}
        self.nsem = 0

    def sb(self, name, shape, dt, es=None):
        self.nsb = getattr(self, "nsb", 0) + 1
        return (es or self.es).enter_context(self.nc.sbuf_tensor("%s_u%d" % (name, self.nsb), shape, dt))

    def I(self, eng, method, reads, writes, **kw):
        return self.P.op(eng, lambda e, kw=kw, m=method: getattr(e, m)(**kw), reads, writes)

    def V(self, method, reads, writes, **kw):
        return self.I("dve", method, reads, writes, **kw)

    def G(self, method, reads, writes, **kw):
        return self.I("pool", method, reads, writes, **kw)

    def A(self, reads, writes, **kw):
        return self.I("act", "activation", reads, writes, **kw)

    def MM(self, reads, writes, **kw):
        return self.I("pe", "matmul", reads, writes, **kw)

    def TR(self, reads, writes, **kw):
        return self.I("pe", "transpose", reads, writes, **kw)

    def DMA(self, q, reads, writes, sem, **kw):
        return self.P.op(q, lambda e, kw=kw: e.dma_start(**kw), reads, writes, dma_sem=sem)

    def rstd(self, src, src_res, tmp, tmp_res, dst, dst_res, n):
        self.A([src_res, "epsc"], [tmp_res], out=tmp, in_=src, func=AF.Sqrt, scale=1.0 / n, bias=self.epsc[0:src.shape[0], :])
        self.V("reciprocal", [tmp_res], [dst_res], out=dst, in_=tmp)

    def barrier(self):
        P = self.P
        allres = list(P.last_w.keys() | P.readers.keys())
        self.MM([], allres + ["BAR"], out=self.ps[7][0:1, 0:8], lhsT=self.identb[0:1, 0:1], rhs=self.identb[0:1, 0:8],
                start=True, stop=True)
        self.A([], allres + ["BAR"], out=self.bar_s[0:1, 0:8], in_=self.bar_s[0:1, 8:16], func=AF.Copy)
        self.V("memset", [], allres + ["BAR"], ap=self.bar_s[0:1, 16:24], constant=0.0)
        self.G("memset", [], allres + ["BAR"], ap=self.bar_s[0:1, 24:32], constant=0.0)
        self.MM(["BAR"], ["BARpe"], out=self.ps[7][0:1, 0:8], lhsT=self.identb[0:1, 0:1], rhs=self.identb[0:1, 0:8],
                start=True, stop=True)
        self.A(["BAR"], ["BARact"], out=self.bar_s[0:1, 0:8], in_=self.bar_s[0:1, 8:16], func=AF.Copy)
        self.V("memset", ["BAR"], ["BARdve"], ap=self.bar_s[0:1, 16:24], constant=0.0)
        self.G("memset", ["BAR"], ["BARpool"], ap=self.bar_s[0:1, 24:32], constant=0.0)
        self.P.op("sp", lambda e: None, ["BAR"], [], track=False)

    def dump(self, name, ap, shape, reads):
        if name not in self.dbg:
            return
        t = self.nc.dram_tensor("dbg_" + name, list(shape), ap.dtype, kind="ExternalOutput").ap()
        self.dbg_outs[name] = t
        self.DMA("sp", reads, [("dbgout", name)], "dbg_" + name, out=t, in_=ap)

    def declare(self):
        nc = self.nc
        dr = self.dram
        dr["x"] = nc.dram_tensor("x", [S, D], F32, kind="ExternalInput").ap()
        for k, shp in WEIGHT_SHAPES.items():
            if self.stop_after is not None and k in ("ffn_w1", "ffn_w2"):
                shp = [NL, 128, 128]
            dr[k] = nc.dram_tensor(k, shp, F32, kind="ExternalInput").ap()
        for k, shp in CONST_SHAPES.items():
            dr[k] = nc.dram_tensor(k, shp, F32, kind="ExternalInput").ap()
        dr["out"] = nc.dram_tensor("out", [S, D], F32, kind="ExternalOutput").ap()
        dr["rs"] = nc.dram_tensor("rs_scratch", [4, 128, 1152], F32, kind="Internal").ap()
        dr["tbl"] = nc.dram_tensor("tbl_scratch", [128, 2528], F32, kind="Internal").ap()

    def build(self):
        with self.es:
            self.declare()
            self._build()
            names = self.P.sem_names()
            sems = {n: self.es.enter_context(self.nc.semaphore("s%d" % i)) for i, n in enumerate(names)}
            self.nsem = len(names)
            self.P.finalize(sems)
        return self.nc

    def _build(self):
        nc, dr = self.nc, self.dram
        sb = self.sb
        self.ps = [self.es.enter_context(nc.psum_tensor("ps%d" % i, [128, 512], F32)) for i in range(8)]
        self.X = sb("X", [128, NT, D], F32)
        self.hT = sb("hT", [128, 8, S], BF16)
        self.Yn = sb("Yn", [128, NT, D], BF16)
        self.identb = sb("identb", [128, 128], BF16)
        self.identf = sb("identf", [128, 128], F32)
        self.bar_s = sb("bar_s", [1, 32], F32)
        self.epsc = sb("epsc", [128, 1], F32)
        self.onec = sb("onec", [128, 1], F32)
        self.zrow = sb("zrow", [1, 512], BF16)
        self.V("memset", [], ["zrow"], ap=self.zrow[:], constant=0.0)
        self.V("memset", [], ["onec"], ap=self.onec[:], constant=1.0)
        self.V("memset", [], ["epsc"], ap=self.epsc[:], constant=EPS)
        self.tri = sb("tri", [128, 128], BF16)
        self.ones = sb("ones", [128, 128], BF16)
        self.mcausal = sb("mcausal", [128, 128], F32)
        self.mstrict = sb("mstrict", [128, 128], F32)
        self.c31 = sb("c31", [128, 4], F32)
        self.gcol = sb("gcol", [128, 8], F32)
        self.cw = sb("cw", [128, 6], F32)
        self.cb = sb("cb", [128, 2], F32)
        self.posT = sb("posT", [128, 32], F32)
        self.stat = sb("stat", [128, 64], F32)

        D_ = self.DMA
        D_("sp", [], ["identf"], "c1", out=self.identf[:], in_=dr["c_ident"][:])
        D_("pool", [], ["identb"], "c2", out=self.identb[:], in_=dr["c_ident"][:])
        D_("pool", [], ["tri"], "c5", out=self.tri[:], in_=dr["c_tri"][:])
        D_("pool", [], ["ones"], "c6", out=self.ones[:], in_=dr["c_ones"][:])
        D_("sp", [], ["mcausal"], "c7", out=self.mcausal[:], in_=dr["c_mcausal"][:])
        D_("sp", [], ["mstrict"], "c8", out=self.mstrict[:], in_=dr["c_mstrict"][:])
        D_("sp", [], ["c31"], "c12", out=self.c31[:], in_=dr["rel_bias"][31:32, :].partition_broadcast(128))
        for i in range(4):
            D_("sp", [], [("X", t) for t in range(4 * i, 4 * i + 4)], "x%d" % i, out=self.X[:, 4 * i:4 * i + 4, :],
               in_=dr["x"][512 * i:512 * (i + 1), :].rearrange("(t p) d -> p t d", p=128))
        self.build_bias_tables()
        for l in range(NL):
            self.layer(l)
            if self.stop_after is not None and self.stop_after[0] == l:
                break
        outs = []
        for t in range(NT):
            D_("sp", [("X", t)], [("out", t)], "out", out=dr["out"][128 * t:128 * (t + 1), :], in_=self.X[:, t, :])
            outs.append(("out", t))
        outs += [("dbgout", n) for n in self.dbg_outs]
        self.P.op("sp", lambda e: None, outs, [], track=False)

    def build_bias_tables(self):
        nc, dr = self.nc, self.dram
        with ExitStack() as es:
            rb = self.sb("rb", [32, 4], F32, es)
            rbm = self.sb("rbm", [32, 4, 128], F32, es)
            oh = self.sb("oh", [32, 1152], F32, es)
            negt = self.sb("negt", [128, 1152], F32, es)
            rrow = self.sb("rrow", [128, 4, 1152], F32, es)
            band = self.sb("band", [128, 4, 16], F32, es)
            tbl = self.sb("tbl_b", [128, 2528], F32, es)
            self.TD = tbl[:, 0:512].rearrange("p (h n) -> p h n", h=4)
            self.TO = tbl[:, 512:1024].rearrange("p (h n) -> p h n", h=4)
            self.TW = tbl[:, 1024:1536].rearrange("p (h n) -> p h n", h=4)
            self.TcFull = tbl[:, 1536:2528].rearrange("p (h n) -> p h n", h=4)
            self.DMA("sp", [], ["rb"], "b1", out=rb[:], in_=dr["rel_bias"][:])
            self.DMA("sp", [], ["oh"], "b2", out=oh[:], in_=dr["c_oh"][:])
            self.DMA("sp", [], ["negt"], "b3", out=negt[:], in_=dr["c_negrow"][:].partition_broadcast(128))
            self.V("tensor_copy", ["rb"], ["rbm"], out=rbm[:], in_=rb[:].unsqueeze(2).to_broadcast([32, 4, 128]))
            for h in range(4):
                for j in range(3):
                    pb = (h * 3 + j) % 2
                    self.MM(["rbm", "oh"], [("ps", pb)], out=self.ps[pb][:, 0:384], lhsT=rbm[:, h, :],
                            rhs=oh[:, 384 * j:384 * (j + 1)], start=True, stop=True)
                    self.V("tensor_tensor", [("ps", pb), "negt"], [("rrow", h)], out=rrow[:, h, 384 * j:384 * (j + 1)],
                           in0=self.ps[pb][:, 0:384], in1=negt[:, 384 * j:384 * (j + 1)], op=ALU.add)
                self.DMA("sp", [("rrow", h)], [("rs", h)], "b4%d" % h, out=dr["rs"][h], in_=rrow[:, h, :])
            rst = dr["rs"].tensor
            for h in range(4):
                base = h * 128 * 1152
                for nm, tl, off in (("TD", self.TD, 127), ("TO", self.TO, 256 + 127), ("TW", self.TW, 512 + 127)):
                    src = bass.AP(tensor=rst, offset=base + off, ap=[[1151, 128], [1, 128]])
                    self.DMA("sp", [("rs", h)], [(nm, h)], "b5%s%d" % (nm, h), out=tl[:, h, :], in_=src)
                src = bass.AP(tensor=rst, offset=base + 768 + 127, ap=[[1151, 128], [16, 16]])
                self.DMA("sp", [("rs", h)], [("band", h)], "b6%d" % h, out=band[:, h, :], in_=src,
                         allow_slow_non_contiguous=True)
            self.V("memset", [], ["TcFull"], ap=self.TcFull, constant=NEG)
            for h in range(4):
                self.V("tensor_scalar", ["c31", "TcFull"], ["TcFull"], out=self.TcFull[:, h, 0:111],
                       in0=self.TcFull[:, h, 0:111], scalar1=0.0, scalar2=self.c31[:, h:h + 1], op0=ALU.mult, op1=ALU.add)
                self.V("tensor_copy", [("band", h), "TcFull"], ["TcFull"], out=self.TcFull[:, h, 111:127], in_=band[:, h, :])
            allt = ["TcFull"] + [(nm, h) for nm in ("TD", "TO", "TW") for h in range(4)]
            self.dump("tbl", tbl[:], [128, 2528], allt)
            self.DMA("sp", allt, ["tbl_dram"], "b7", out=dr["tbl"][:], in_=tbl[:])
            self.barrier()

    def load_w(self, l, es, c0, ncols):
        w = self.sb("wsl", [128, 8, ncols], BF16, es)
        src = self.dram["w_in"][l, :, c0:c0 + ncols].rearrange("(c p) n -> p c n", p=128)
        for half in range(2):
            self.DMA("pool", [], [("wsl", half)], "wb%d" % half,
                     out=w[:, 4 * half:4 * half + 4, :], in_=src[:, 4 * half:4 * half + 4, :])
        return w, [("wsl", 0), ("wsl", 1)]

    def layer(self, l):
        dr = self.dram
        D_ = self.DMA

        def col(ap1d, n):
            return bass.AP(tensor=ap1d.tensor, offset=ap1d.offset, ap=[[1, n], [1, 1]])
        k = 0
        for ci, src, n in ((0, dr["nsa_q_norm"][l], 64), (1, dr["nsa_k_norm"][l, 1], 64), (2, dr["nsa_k_norm"][l, 2], 64),
                           (3, dr["nsa_k_norm"][l, 0], 64)):
            for half in range(2):
                D_("sp", [], [("gcol", ci, half)], "g%d%d" % (ci, half), out=self.gcol[64 * half:64 * half + 64, ci:ci + 1],
                   in_=col(src, 64))
        D_("sp", [], [("gcol", 4, 0)], "g40", out=self.gcol[0:96, 4:5], in_=col(dr["mla_q_norm"][l], 96))
        D_("sp", [], [("gcol", 5, 0)], "g50", out=self.gcol[0:96, 5:6], in_=col(dr["mla_k_norm"][l], 96))
        cwl = dr["conv_w"][l]
        D_("sp", [], ["cw"], "p6", out=self.cw[:], in_=bass.AP(tensor=cwl.tensor, offset=cwl.offset, ap=[[1, 128], [128, 6]]),
           allow_slow_non_contiguous=True)
        D_("sp", [], ["cb"], "p7", out=self.cb[:], in_=dr["conv_b"][l].rearrange("(c p) -> p c", p=128),
           allow_slow_non_contiguous=True)
        for kv in range(2):
            D_("sp", [], [("posT", kv)], "p8%d" % kv, out=self.posT[64 * kv:64 * kv + 64, :],
               in_=dr["cmp_pos"][l, kv].rearrange("i d -> d i"), allow_slow_non_contiguous=True)
        self.norm_transpose(l, "norm1_w", "n1t")
        self.mixer_a(l)
        self.barrier()
        if self.stop_after == (l, "a"):
            return
        self.mixer_sb(l)
        self.barrier()
        if self.stop_after == (l, "sb"):
            return
        self.mixer_mla(l)
        self.barrier()
        if self.stop_after == (l, "mla"):
            return
        self.mixer_nsa(l)
        self.barrier()
        if self.stop_after in ((l, "nsa"), (l, "nsa1"), (l, "nsa2"), (l, "nsa3")):
            return
        self.out_proj(l)
        self.barrier()
        if self.stop_after == (l, "out"):
            return
        self.norm_transpose(l, "norm2_w", "n2t")
        self.ffn(l)
        self.barrier()

    def norm_transpose(self, l, wname, gname):
        X, hT, st = self.X, self.hT, self.stat
        with ExitStack() as es:
            gt = self.sb("nt_g", [128, D], F32, es)
            self.DMA("sp", [], [gname], "p1", out=gt[:], in_=self.dram[wname][l:l + 1, :].partition_broadcast(128))
            junk = self.sb("nt_junk", [128, D], BF16, es)
            hn = [self.sb("nt_hn%d" % i, [128, D], BF16, es) for i in range(2)]
            for t in range(NT):
                b = t % 2
                self.A([("X", t)], ["nt_junk", ("nt_ssq", b)], out=junk[:], in_=X[:, t, :], func=AF.Square,
                       accum_out=st[:, b:b + 1])
                self.rstd(st[:, b:b + 1], ("nt_ssq", b), st[:, 2 + b:3 + b], ("nt_r", b), st[:, 4 + b:5 + b], ("nt_rs", b), D)
                self.V("scalar_tensor_tensor", [("X", t), ("nt_rs", b), gname], [("nt_hn", b)], out=hn[b][:], in0=X[:, t, :],
                       scalar=st[:, 4 + b:5 + b], in1=gt[:], op0=ALU.mult, op1=ALU.mult)
                pb = 6 + b
                pv = self.ps[pb][:].bitcast(BF16)
                for c in range(8):
                    self.TR([("nt_hn", b), "identb"], [("ps", pb)], out=pv[:, 128 * c:128 * (c + 1)],
                            in_=hn[b][:, 128 * c:128 * (c + 1)], identity=self.identb[:])
                eng = "act" if t % 2 == 0 else "dve"
                outap = hT[:, :, 128 * t:128 * (t + 1)]
                inap = pv.rearrange("p (c n) -> p c n", c=8)
                if eng == "act":
                    self.A([("ps", pb)], [("hT", t)], out=outap, in_=inap, func=AF.Copy)
                else:
                    self.V("tensor_copy", [("ps", pb)], [("hT", t)], out=outap, in_=inap)
        self.barrier()

    def hT_res(self, tb):
        return [("hT", t) for t in range(4 * tb, 4 * tb + 4)]

    def group_norm(self, yap, yres, g, t, slot):
        st = self.stat
        o = 8 + 4 * slot
        junk = self.gn_junk[slot]
        self.A(yres, [("gn_junk", slot), ("gn_ssq", slot)], out=junk[:], in_=yap, func=AF.Square,
               accum_out=st[:, o:o + 1])
        self.rstd(st[:, o:o + 1], ("gn_ssq", slot), st[:, o + 1:o + 2], ("gn_r", slot), st[:, o + 2:o + 3], ("gn_rs", slot), 256)
        self.V("tensor_scalar", yres + [("gn_rs", slot)], [("Yn", t, g)],
               out=self.Yn[:, t, 256 * g:256 * (g + 1)], in0=yap, scalar1=st[:, o + 2:o + 3], scalar2=None, op0=ALU.mult)

    def mixer_a(self, l):
        hT = self.hT
        with ExitStack() as es:
            w, wr = self.load_w(l, es, C_A, 768)
            self.gn_junk = [self.sb("gn_junk%d" % i, [128, 256], BF16, es) for i in range(2)]
            v = self.sb("a_v", [128, 2, S + 2], F32, es)
            cs = [self.sb("a_cs%d" % i, [128, 512], F32, es) for i in range(2)]
            y = [self.sb("a_y%d" % i, [128, 512], F32, es) for i in range(2)]
            ya = [self.sb("a_ya%d" % i, [128, 2, 512], F32, es) for i in range(2)]
            self.V("memset", [], [("a_v", 0), ("a_v", 1)], ap=v[:, :, 0:2], constant=0.0)
            for tb in range(4):
                hres = self.hT_res(tb)
                yb = tb % 2
                for c in range(2):
                    pbn = [0 + 3 * c, 1 + 3 * c, 2 + 3 * c]
                    for gi, co in enumerate((0, 256, 512)):
                        pb = pbn[gi]
                        for dc in range(8):
                            self.MM(hres + wr, [("ps", pb)], out=self.ps[pb][:, :],
                                    lhsT=w[:, dc, co + 128 * c:co + 128 * (c + 1)], rhs=hT[:, dc, 512 * tb:512 * (tb + 1)],
                                    start=(dc == 0), stop=(dc == 7))
                    pbB, pbC, pbU = pbn
                    self.A([("ps", pbC)], [("a_cs", c)], out=cs[c][:], in_=self.ps[pbC][:], func=AF.Copy)
                    vs = v[:, c, 2 + 512 * tb:2 + 512 * (tb + 1)]
                    self.V("tensor_tensor", [("ps", pbU), ("a_cs", c)], [("a_v", c)], out=vs, in0=self.ps[pbU][:], in1=cs[c][:],
                           op=ALU.mult)
                    self.V("tensor_scalar", [("a_v", c), "cw", "cb"], [("a_y", c)], out=y[c][:], in0=vs,
                           scalar1=self.cw[:, 4 + c:5 + c], scalar2=self.cb[:, c:c + 1], op0=ALU.mult, op1=ALU.add)
                    self.V("scalar_tensor_tensor", [("a_v", c), "cw", ("a_y", c)], [("a_y", c)], out=y[c][:],
                           in0=v[:, c, 1 + 512 * tb:1 + 512 * (tb + 1)], scalar=self.cw[:, 2 + c:3 + c], in1=y[c][:],
                           op0=ALU.mult, op1=ALU.add)
                    self.V("scalar_tensor_tensor", [("a_v", c), "cw", ("a_y", c)], [("a_y", c)], out=y[c][:],
                           in0=v[:, c, 512 * tb:512 * (tb + 1)], scalar=self.cw[:, c:c + 1], in1=y[c][:],
                           op0=ALU.mult, op1=ALU.add)
                    self.V("tensor_tensor", [("ps", pbB), ("a_y", c)], [("a_ya", yb, c)], out=ya[yb][:, c, :],
                           in0=self.ps[pbB][:], in1=y[c][:], op=ALU.mult)
                for j in range(4):
                    t = 4 * tb + j
                    pb = 6 + (j % 2)
                    for c in range(2):
                        self.TR([("a_ya", yb, c), "identf"], [("ps", pb)], out=self.ps[pb][:, 128 * c:128 * (c + 1)],
                                in_=ya[yb][:, c, 128 * j:128 * (j + 1)], identity=self.identf[:])
                    self.group_norm(self.ps[pb][:, 0:256], [("ps", pb)], 0, t, j % 2)
            self.dump("yn_a", self.Yn[:, :, 0:256], [128, NT, 256], [("Yn", t, 0) for t in range(NT)])

    def pipeline(self, items, stages):
        n = len(items)
        ns = len(stages)
        for step in range(n + ns - 1):
            for si, st in enumerate(stages):
                i = step - si
                if 0 <= i < n:
                    st(i, items[i])

    def proj_feat(self, w, wr, col0, tb, pb):
        for dc in range(8):
            self.MM(self.hT_res(tb) + wr, [("ps", pb)], out=self.ps[pb][:, :], lhsT=w[:, dc, col0:col0 + 128],
                    rhs=self.hT[:, dc, 512 * tb:512 * (tb + 1)], start=(dc == 0), stop=(dc == 7))

    def proj_tok(self, w, wr, col0, ncol, t, pb):
        for dc in range(8):
            self.MM([("hT", t)] + wr, [("ps", pb)], out=self.ps[pb][:, 0:ncol], lhsT=self.hT[:, dc, 128 * t:128 * (t + 1)],
                    rhs=w[:, dc, col0:col0 + ncol], start=(dc == 0), stop=(dc == 7))

    def mixer_sb(self, l):
        with ExitStack() as es:
            sb = lambda n, shp, dt: self.sb(n, shp, dt, es)
            w, wr = self.load_w(l, es, C_S, 768)
            self.gn_junk = [sb("gn_junk%d" % i, [128, 256], BF16) for i in range(2)]
            sqT = sb("s_qT", [128, 2, S], BF16)
            skT = sb("s_kT", [128, 2, S], BF16)
            sv = sb("s_v", [128, NT, 256], BF16)
            NB = 3
            e_t = [sb("s_e%d" % i, [128, 512], F32) for i in range(NB)]
            LTb = [sb("s_L%d" % i, [128, 512], BF16) for i in range(NB)]
            ec = [sb("s_ec%d" % i, [128, 512], BF16) for i in range(NB)]
            aT = [sb("s_a%d" % i, [128, 512], BF16) for i in range(NB)]
            Sf = sb("s_Sf", [128, 512], F32)
            Sb = [sb("s_Sb%d" % i, [128, 512], BF16) for i in range(2)]
            Ysb = sb("s_Y", [128, 4, 256], F32)
            k = 0
            for tb in range(4):
                for gi in range(2):
                    for c in range(2):
                        pb = k % 4
                        k += 1
                        self.proj_feat(w, wr, gi * 256 + c * 128, tb, pb)
                        dst = (sqT if gi == 0 else skT)[:, c, 512 * tb:512 * (tb + 1)]
                        dres = ("s_qT" if gi == 0 else "s_kT", c, tb)
                        if k % 2 == 0:
                            self.A([("ps", pb)], [dres], out=dst, in_=self.ps[pb][:], func=AF.Copy)
                        else:
                            self.V("tensor_copy", [("ps", pb)], [dres], out=dst, in_=self.ps[pb][:])
            for t in range(NT):
                pb = 4 + t % 2
                self.proj_tok(w, wr, 512, 256, t, pb)
                if t % 2 == 0:
                    self.A([("ps", pb)], [("s_v", t)], out=sv[:, t, :], in_=self.ps[pb][:, 0:256], func=AF.Copy)
                else:
                    self.V("tensor_copy", [("ps", pb)], [("s_v", t)], out=sv[:, t, :], in_=self.ps[pb][:, 0:256])
            items = []
            g = 0
            for qb in range(4):
                for h in range(4):
                    nk = 4 * qb + 4
                    for i, kt in enumerate(range(nk - 1, -1, -1)):
                        items.append(dict(qb=qb, h=h, kt=kt, first=(i == 0), last=(kt == 0), g=g))
                    g += 1

            def geom(it):
                qb, kt = it["qb"], it["kt"]
                f = max(kt, 4 * qb)
                c0 = (f - 4 * qb) * 128
                return f, c0, kt >= 4 * qb

            def st1(i, it):
                qb, h, kt = it["qb"], it["h"], it["kt"]
                f, c0, diag = geom(it)
                c, p0 = h // 2, 64 * (h % 2)
                b = i % NB
                zb = i % 2
                qres = [("s_qT", c, qb)]
                self.MM(qres + [("s_kT", c, kt // 4)], [("ps", zb)], out=self.ps[zb][:, c0:512],
                        lhsT=skT[p0:p0 + 64, c, 128 * kt:128 * (kt + 1)], rhs=sqT[p0:p0 + 64, c, 512 * qb + c0:512 * (qb + 1)],
                        start=True, stop=True)
                self.A([("ps", zb)], [("s_e", b)], out=e_t[b][:, c0:512], in_=self.ps[zb][:, c0:512], func=AF.Exp, scale=0.125)
                self.A([("s_e", b), "onec"], [("s_L", b)], out=LTb[b][:, c0:512], in_=e_t[b][:, c0:512], func=AF.Ln, bias=self.onec[:, :])
                if diag:
                    self.V("tensor_tensor", [("s_L", b), "mstrict"], [("s_L", b)], out=LTb[b][:, c0:c0 + 128],
                           in0=LTb[b][:, c0:c0 + 128], in1=self.mstrict[:], op=ALU.mult)

            def st2(i, it):
                f, c0, diag = geom(it)
                b = i % NB
                cb_ = 2 + i % 2
                first = it["first"]
                self.MM([("s_L", b), "tri"], [("ps", cb_)], out=self.ps[cb_][:, c0:512], lhsT=self.tri[:], rhs=LTb[b][:, c0:512],
                        start=True, stop=first)
                if not first:
                    self.MM([("s_Sb", i % 2), "ones"], [("ps", cb_)], out=self.ps[cb_][:, c0:512], lhsT=self.ones[:],
                            rhs=Sb[i % 2][:, c0:512], start=False, stop=True)
                self.A([("ps", cb_)], [("s_ec", b)], out=ec[b][:, c0:512], in_=self.ps[cb_][:, c0:512], func=AF.Exp, scale=-1.0)
                self.V("tensor_tensor", [("s_e", b), ("s_ec", b)], [("s_a", b)], out=aT[b][:, c0:512], in0=e_t[b][:, c0:512],
                       in1=ec[b][:, c0:512], op=ALU.mult)
                if diag:
                    self.V("tensor_tensor", [("s_a", b), "mstrict"], [("s_a", b)], out=aT[b][:, c0:c0 + 128],
                           in0=aT[b][:, c0:c0 + 128], in1=self.mstrict[:], op=ALU.mult)
                if not it["last"]:
                    if first:
                        self.G("memset", [], ["s_Sf"], ap=Sf[:], constant=0.0)
                    self.G("tensor_tensor", ["s_Sf", ("s_L", b)], ["s_Sf"], out=Sf[:, c0:512], in0=Sf[:, c0:512],
                           in1=LTb[b][:, c0:512], op=ALU.add)
                    self.G("tensor_copy", ["s_Sf"], [("s_Sb", (i + 1) % 2)], out=Sb[(i + 1) % 2][:], in_=Sf[:])

            def st3(i, it):
                qb, h, kt = it["qb"], it["h"], it["kt"]
                f, c0, diag = geom(it)
                b = i % NB
                acc = 4 + it["g"] % 2
                if it["first"]:
                    self.MM(["zrow"], [("ps", acc)], out=self.ps[acc][:, 0:256], lhsT=self.zrow[0:1, 0:128], rhs=self.zrow[0:1, 0:256],
                            start=True, stop=False)
                for j in range(f - 4 * qb, 4):
                    self.MM([("s_a", b), ("s_v", kt)], [("ps", acc)], out=self.ps[acc][:, 64 * j:64 * (j + 1)],
                            lhsT=aT[b][:, 128 * j:128 * (j + 1)], rhs=sv[:, kt, 64 * h:64 * (h + 1)],
                            start=False, stop=(kt == 0 and j == 3))
                if it["last"]:
                    self.A([("ps", acc)], [("s_Y", j, h) for j in range(4)], out=Ysb[:, :, 64 * h:64 * (h + 1)],
                           in_=self.ps[acc][:, 0:256].rearrange("p (j d) -> p j d", j=4), func=AF.Copy)
                    if h == 3:
                        for j in range(4):
                            self.group_norm(Ysb[:, j, :], [("s_Y", j, hh) for hh in range(4)], 3, 4 * qb + j, j % 2)

            self.pipeline(items, [st1, st2, st3])
            self.dump("yn_sb", self.Yn[:, :, 768:1024], [128, NT, 256], [("Yn", t, 3) for t in range(NT)])

    def attn_bufs(self, es, tag):
        pT = [self.sb("%s_p%d" % (tag, i), [128, 512], BF16, es) for i in range(3)]
        dtmp = [self.sb("%s_d%d" % (tag, i), [128, 512], F32, es) for i in range(2)]
        return pT, dtmp

    def attn_core(self, bufs, tag, nheads_K, items, score_fn, bias_fn, scale, v_fn, finish_fn, cbias=None):
        NB = 3
        pT, dtmp = bufs

        def st1(i, it):
            zb = i % 2
            b = i % 2
            tiles = it["tiles"]
            c0 = 128 * tiles[0][0]
            c1 = 128 * (tiles[-1][0] + 1)
            score_fn(it, zb, c0, c1)
            for j, kind in tiles:
                if kind is not None:
                    bap, bres = kind
                    self.V("scalar_tensor_tensor", [("ps", zb)] + bres, [(tag + "_d", b, j)], out=dtmp[b][:, 128 * j:128 * (j + 1)],
                           in0=self.ps[zb][:, 128 * j:128 * (j + 1)], scalar=scale, in1=bap, op0=ALU.mult, op1=ALU.add)

        def st2(i, it):
            zb = i % 2
            b = i % NB
            b2 = i % 2
            tiles = it["tiles"]
            run = []
            runs = []
            for j, kind in tiles:
                if kind is None:
                    run.append(j)
                else:
                    if run:
                        runs.append(run)
                        run = []
                    self.A([(tag + "_d", b2, j)], [(tag + "_p", b, j)], out=pT[b][:, 128 * j:128 * (j + 1)],
                           in_=dtmp[b2][:, 128 * j:128 * (j + 1)], func=AF.Exp)
            if run:
                runs.append(run)
            for r in runs:
                a0, a1 = 128 * r[0], 128 * (r[-1] + 1)
                kw = dict(out=pT[b][:, a0:a1], in_=self.ps[zb][:, a0:a1], func=AF.Exp, scale=scale)
                rr = [("ps", zb)]
                if cbias is not None:
                    kw["bias"] = cbias(it)
                    rr.append("c31")
                self.A(rr, [(tag + "_p", b, j) for j in r], **kw)

        def st3(i, it):
            b = i % NB
            acc = 4 + it["g"] % 2
            vap, vres, vw = v_fn(it)
            if it["gfirst"]:
                self.MM(["zrow"], [("ps", acc)], out=self.ps[acc][:, 0:4 * vw], lhsT=self.zrow[0:1, 0:128], rhs=self.zrow[0:1, 0:4 * vw],
                        start=True, stop=False)
            for j, kind in it["tiles"]:
                self.MM([(tag + "_p", b, j)] + vres, [("ps", acc)], out=self.ps[acc][:, vw * j:vw * (j + 1)],
                        lhsT=pT[b][:, 128 * j:128 * (j + 1)], rhs=vap, start=False, stop=(it["last"] and j == it["tiles"][-1][0]))
            if it["last"]:
                finish_fn(it, acc)

        self.pipeline(items, [st1, st2, st3])

    def rope(self, x1, x2, t, nh, tmp, res_in, res_tmp):
        shp = [128, nh, 16]
        cb = self.cos[:, t, :].unsqueeze(1).to_broadcast(shp)
        sb_ = self.sin[:, t, :].unsqueeze(1).to_broadcast(shp)
        t1, t2, t3, t4 = [tmp[:, i, 0:nh, :] for i in range(4)]
        rd = res_in + ["cos", "sin"]
        self.V("tensor_tensor", rd, [res_tmp + "1"], out=t1, in0=x1, in1=cb, op=ALU.mult)
        self.V("tensor_tensor", rd, [res_tmp + "2"], out=t2, in0=x2, in1=sb_, op=ALU.mult)
        self.V("tensor_tensor", rd, [res_tmp + "3"], out=t3, in0=x1, in1=sb_, op=ALU.mult)
        self.V("tensor_tensor", rd, [res_tmp + "4"], out=t4, in0=x2, in1=cb, op=ALU.mult)
        self.V("tensor_tensor", [res_tmp + "1", res_tmp + "2"], res_in, out=x1, in0=t1, in1=t2, op=ALU.subtract)
        self.V("tensor_tensor", [res_tmp + "3", res_tmp + "4"], res_in, out=x2, in0=t3, in1=t4, op=ALU.add)

    def head_norm_T(self, src, src_res, nh, hd, t, dstT, dst_res, gidx, sq, qn, stc, pb, tag):
        st = self.stat
        self.V("tensor_tensor", src_res, [tag + "_sq"], out=sq[:, 0:nh, 0:hd], in0=src, in1=src, op=ALU.mult)
        self.V("tensor_reduce", [tag + "_sq"], [tag + "_ss"], out=st[:, stc:stc + nh], in_=sq[:, 0:nh, 0:hd], axis=AX.X, op=ALU.add)
        self.rstd(st[:, stc:stc + nh], tag + "_ss", st[:, stc + 4:stc + 4 + nh], tag + "_r", st[:, stc + 8:stc + 8 + nh], tag + "_rs", hd)
        self.V("tensor_tensor", src_res + [tag + "_rs"], [tag + "_n"], out=qn[:, 0:nh, 0:hd], in0=src,
               in1=st[:, stc + 8:stc + 8 + nh].unsqueeze(2).to_broadcast([128, nh, hd]), op=ALU.mult)
        pv = self.ps[pb][:].bitcast(BF16)
        for h in range(nh):
            self.TR([tag + "_n", "identb"], [("ps", pb)], out=pv[0:hd, 128 * h:128 * (h + 1)], in_=qn[:, h, 0:hd],
                    identity=self.identb[:])
        self.V("tensor_scalar", [("ps", pb), ("gcol", gidx, 0)], [dst_res], out=dstT[0:hd, :, 128 * t:128 * (t + 1)],
               in0=pv[0:hd, 0:128 * nh].rearrange("p (h n) -> p h n", h=nh), scalar1=self.gcol[0:hd, gidx:gidx + 1], scalar2=None,
               op0=ALU.mult)

    def mixer_mla(self, l):
        dr = self.dram
        st = self.stat
        with ExitStack() as es:
            sb = lambda n, shp, dt: self.sb(n, shp, dt, es)
            self.gn_junk = [sb("gn_junk%d" % i, [128, 256], BF16) for i in range(2)]
            abufs = self.attn_bufs(es, "m")
            wqb = sb("m_wqb", [128, 2, 384], BF16)
            wkvb = sb("m_wkvb", [128, 512], BF16)
            self.DMA("pool", [], ["m_wqb0"], "mw0", out=wqb[:, 0, :], in_=dr["mla_wq_b"][l, 0:128, :])
            self.DMA("pool", [], ["m_wqb1"], "mw1", out=wqb[0:64, 1, :], in_=dr["mla_wq_b"][l, 128:192, :])
            self.DMA("pool", [], ["m_wkvb"], "mw2", out=wkvb[:], in_=dr["mla_wkv_b"][l])
            qmT = sb("m_qT", [128, 4, S], BF16)
            kmT = sb("m_kT", [128, 4, S], BF16)
            vma = sb("m_v", [128, NT, 4, 65], BF16)
            Ysb = sb("m_Y", [128, 4, 256], F32)
            self.V("memset", [], [("m_v", t) for t in range(NT)], ap=vma[:, :, :, 64:65], constant=1.0)
            with ExitStack() as es2:
                sb2 = lambda n, shp, dt: self.sb(n, shp, dt, es2)
                w, wr = self.load_w(l, es2, C_M, 352)
                self.cos = sb2("cos", [128, NT, 16], F32)
                self.sin = sb2("sin", [128, NT, 16], F32)
                self.qat = sb2("qat", [128, 192], F32)
                self.kvt = sb2("kvt", [128, 128], F32)
                self.DMA("sp", [], ["cos"], "c3", out=self.cos[:], in_=dr["c_cos"][:])
                self.DMA("sp", [], ["sin"], "c4", out=self.sin[:], in_=dr["c_sin"][:])
                self.DMA("sp", [], ["qat"], "p4", out=self.qat[:], in_=dr["mla_q_a_norm"][l:l + 1, :].partition_broadcast(128))
                self.DMA("sp", [], ["kvt"], "p5", out=self.kvt[:], in_=dr["mla_kv_norm"][l:l + 1, :].partition_broadcast(128))
                junk = sb2("m_junk", [128, 192], BF16)
                cqn = sb2("m_cqn", [128, 192], BF16)
                ckvn = sb2("m_ckvn", [128, 128], BF16)
                krs = sb2("m_krs", [128, 1, 32], F32)
                cT = sb2("m_cT", [128, 3, 128], BF16)
                qs = sb2("m_qs", [128, 4, 96], F32)
                ks_ = sb2("m_ks", [128, 4, 96], F32)
                sq = sb2("m_sq", [128, 4, 96], F32)
                qn = sb2("m_qn", [128, 4, 96], BF16)
                kn = sb2("m_kn", [128, 4, 96], BF16)
                rtmp = sb2("m_rt", [128, 4, 4, 16], F32)
                for t in range(NT):
                    self.proj_tok(w, wr, 0, 352, t, 0)
                    pm = self.ps[0]
                    self.A([("ps", 0)], ["m_junk", "m_ssq0"], out=junk[:, 0:192], in_=pm[:, 0:192], func=AF.Square,
                           accum_out=st[:, 20:21])
                    self.A([("ps", 0)], ["m_junk", "m_ssq1"], out=junk[:, 0:128], in_=pm[:, 192:320], func=AF.Square,
                           accum_out=st[:, 21:22])
                    self.rstd(st[:, 20:21], "m_ssq0", st[:, 22:23], "m_r0", st[:, 24:25], "m_rs0", 192)
                    self.rstd(st[:, 21:22], "m_ssq1", st[:, 23:24], "m_r1", st[:, 25:26], "m_rs1", 128)
                    self.V("scalar_tensor_tensor", [("ps", 0), "m_rs0", "qat"], ["m_cqn"], out=cqn[:], in0=pm[:, 0:192],
                           scalar=st[:, 24:25], in1=self.qat[:], op0=ALU.mult, op1=ALU.mult)
                    self.V("scalar_tensor_tensor", [("ps", 0), "m_rs1", "kvt"], ["m_ckvn"], out=ckvn[:], in0=pm[:, 192:320],
                           scalar=st[:, 25:26], in1=self.kvt[:], op0=ALU.mult, op1=ALU.mult)
                    self.A([("ps", 0)], ["m_krs"], out=krs[:, 0, :], in_=pm[:, 320:352], func=AF.Copy)
                    pv1 = self.ps[1][:].bitcast(BF16)
                    self.TR(["m_cqn", "identb"], [("ps", 1)], out=pv1[:, 0:128], in_=cqn[:, 0:128], identity=self.identb[:])
                    self.TR(["m_cqn", "identb"], [("ps", 1)], out=pv1[0:64, 128:256], in_=cqn[:, 128:192], identity=self.identb[:])
                    self.TR(["m_ckvn", "identb"], [("ps", 1)], out=pv1[:, 256:384], in_=ckvn[:], identity=self.identb[:])
                    self.A([("ps", 1)], ["m_cT"], out=cT[:], in_=pv1[:, 0:384].rearrange("p (c n) -> p c n", c=3), func=AF.Copy)
                    self.MM(["m_cT", "m_wqb0"], [("ps", 2)], out=self.ps[2][:, 0:384], lhsT=cT[:, 0, :], rhs=wqb[:, 0, :],
                            start=True, stop=False)
                    self.MM(["m_cT", "m_wqb1"], [("ps", 2)], out=self.ps[2][:, 0:384], lhsT=cT[0:64, 1, :], rhs=wqb[0:64, 1, :],
                            start=False, stop=True)
                    self.MM(["m_cT", "m_wkvb"], [("ps", 3)], out=self.ps[3][:, 0:512], lhsT=cT[:, 2, :], rhs=wkvb[:],
                            start=True, stop=True)
                    self.A([("ps", 2)], ["m_qs"], out=qs[:], in_=self.ps[2][:, 0:384].rearrange("p (h d) -> p h d", h=4), func=AF.Copy)
                    self.rope(qs[:, :, 64:80], qs[:, :, 80:96], t, 4, rtmp, ["m_qs"], "m_rt")
                    self.head_norm_T(qs[:], ["m_qs"], 4, 96, t, qmT, ("m_qT", t), 4, sq, qn, 28, 4, "m_q")
                    kvv = self.ps[3][:, 0:512].rearrange("p (h d) -> p h d", h=4)
                    self.rope(krs[:, :, 0:16], krs[:, :, 16:32], t, 1, rtmp, ["m_krs"], "m_rk")
                    self.V("tensor_copy", [("ps", 3)], ["m_ksA"], out=ks_[:, :, 0:64], in_=kvv[:, :, 0:64])
                    self.V("tensor_copy", ["m_krs"], ["m_ksB"], out=ks_[:, :, 64:96], in_=krs[:, 0:1, :].to_broadcast([128, 4, 32]))
                    self.head_norm_T(ks_[:], ["m_ksA", "m_ksB"], 4, 96, t, kmT, ("m_kT", t), 5, sq, kn, 44, 5, "m_k")
                    self.A([("ps", 3)], [("m_v", t)], out=vma[:, t, :, 0:64], in_=kvv[:, :, 64:128], func=AF.Copy)
            self.dump("m_qT", qmT[0:96, :, :], [96, 4, S], [("m_qT", t) for t in range(NT)])
            self.dump("m_kT", kmT[0:96, :, :], [96, 4, S], [("m_kT", t) for t in range(NT)])
            scale = 96 ** -0.5
            items = []
            g = 0
            for qb in range(4):
                for h in range(4):
                    nk = 4 * qb + 4
                    for kt in range(nk):
                        f = max(kt, 4 * qb)
                        tiles = [(j, ((self.mcausal[:], ["mcausal"]) if 4 * qb + j == kt else None)) for j in range(f - 4 * qb, 4)]
                        items.append(dict(qb=qb, h=h, kt=kt, g=g, last=(kt == nk - 1), gfirst=(kt == 0), tiles=tiles,
                                          start={j: kt == 0 for j in range(4)}, stop={j: kt == 4 * qb + j for j in range(4)}))
                    g += 1

            def score_fn(it, zb, c0, c1):
                qb, h, kt = it["qb"], it["h"], it["kt"]
                self.MM([("m_qT", t) for t in range(4 * qb, 4 * qb + 4)] + [("m_kT", kt)], [("ps", zb)], out=self.ps[zb][:, c0:c1],
                        lhsT=kmT[0:96, h, 128 * kt:128 * (kt + 1)], rhs=qmT[0:96, h, 512 * qb + c0:512 * qb + c1],
                        start=True, stop=True)

            def v_fn(it):
                return vma[:, it["kt"], it["h"], :], [("m_v", it["kt"])], 65

            def finish(it, acc):
                qb, h = it["qb"], it["h"]
                av = self.ps[acc][:, 0:260].rearrange("p (j d) -> p j d", j=4)
                self.V("tensor_scalar", [("ps", acc)], ["m_rsum"], out=st[:, 60:64], in0=av[:, :, 64], scalar1=1e-30, scalar2=None,
                       op0=ALU.max)
                self.V("reciprocal", ["m_rsum"], ["m_rinv"], out=st[:, 56:60], in_=st[:, 60:64])
                self.V("tensor_tensor", [("ps", acc), "m_rinv"], [("m_Y", j, h) for j in range(4)], out=Ysb[:, :, 64 * h:64 * (h + 1)],
                       in0=av[:, :, 0:64], in1=st[:, 56:60].unsqueeze(2).to_broadcast([128, 4, 64]), op=ALU.mult)
                if h == 3:
                    for j in range(4):
                        self.group_norm(Ysb[:, j, :], [("m_Y", j, hh) for hh in range(4)], 2, 4 * qb + j, j % 2)

            self.attn_core(abufs, "m", 96, items, score_fn, None, scale, v_fn, finish)
            self.dump("yn_mla", self.Yn[:, :, 512:768], [128, NT, 256], [("Yn", t, 2) for t in range(NT)])

    def mixer_nsa(self, l):
        dr = self.dram
        st = self.stat
        with ExitStack() as es:
            sb = lambda n, shp, dt: self.sb(n, shp, dt, es)
            self.gn_junk = [sb("gn_junk%d" % i, [128, 256], BF16) for i in range(2)]
            abufs = self.attn_bufs(es, "n")
            tbl = sb("n_tbl", [128, 2528], F32)
            self.DMA("sp", ["tbl_dram"], ["n_tbl"], "nt1", out=tbl[:], in_=dr["tbl"][:])
            TD = tbl[:, 0:512].rearrange("p (h n) -> p h n", h=4)
            TO = tbl[:, 512:1024].rearrange("p (h n) -> p h n", h=4)
            TW = tbl[:, 1024:1536].rearrange("p (h n) -> p h n", h=4)
            TcFull = tbl[:, 1536:2528].rearrange("p (h n) -> p h n", h=4)
            E = sb("n_E", [128, S], BF16)
            vm = sb("n_vm", [128, NT, 32], F32)
            ad = sb("n_ad", [128, NT, 32], F32)
            self.DMA("pool", [], [("n_E", 0)], "nt2", out=E[0:32, :], in_=dr["c_e"][:])
            self.DMA("pool", [], [("n_E", 1)], "nt5", out=E[64:96, :], in_=dr["c_e"][:])
            self.DMA("sp", [], ["n_vm"], "nt3", out=vm[:], in_=dr["c_vm"][:])
            self.DMA("sp", [], ["n_ad"], "nt4", out=ad[:], in_=dr["c_ad"][:])
            qT = sb("n_qT", [128, 2, S], BF16)
            ksT = sb("n_ksT", [128, S], BF16)
            kwT = sb("n_kwT", [128, S], BF16)
            vsa = sb("n_vs", [128, NT, 65], BF16)
            vwa = sb("n_vw", [128, NT, 65], BF16)
            gates = sb("n_g", [128, NT, 12], F32)
            kcT = sb("n_kcT", [128, 128], BF16)
            vcs = sb("n_vc", [128, 64], BF16)
            self.V("memset", [], [("n_vs", t) for t in range(NT)], ap=vsa[:, :, 64:65], constant=1.0)
            self.V("memset", [], [("n_vw", t) for t in range(NT)], ap=vwa[:, :, 64:65], constant=1.0)
            with ExitStack() as es_kv:
              kvcT = self.sb("n_kvcT", [128, S], F32, es_kv)
              junk = self.sb("n_junk", [128, 64], BF16, es_kv)
              with ExitStack() as es2:
                sb2 = lambda n, shp, dt: self.sb(n, shp, dt, es2)
                w, wr = self.load_w(l, es2, C_N, 652)
                sqq = sb2("n_sqq", [128, 4, 64], F32)
                qn = sb2("n_qn", [128, 4, 64], BF16)
                kd = sb2("n_kd", [128, 2, 2, 64], BF16)
                gtmp = sb2("n_gtmp", [128, 12], F32)
                for tb in range(4):
                    pb = tb % 2
                    self.proj_feat(w, wr, 256, tb, pb)
                    self.A([("ps", pb)], [("n_kvcT", tb)], out=kvcT[:, 512 * tb:512 * (tb + 1)], in_=self.ps[pb][:], func=AF.Copy)
                for t in range(NT):
                    self.proj_tok(w, wr, 0, 256, t, 2)
                    self.proj_tok(w, wr, 384, 268, t, 3)
                    p2, p3 = self.ps[2], self.ps[3]
                    qv = p2[:, 0:256].rearrange("p (h d) -> p h d", h=4)
                    self.A([("ps", 2)], ["n_sqq"], out=sqq[:], in_=qv, func=AF.Square)
                    self.V("tensor_reduce", ["n_sqq"], ["n_qss"], out=st[:, 20:24], in_=sqq[:], axis=AX.X, op=ALU.add)
                    self.rstd(st[:, 20:24], "n_qss", st[:, 24:28], "n_qr", st[:, 28:32], "n_qrs", 64)
                    self.V("tensor_tensor", [("ps", 2), "n_qrs"], ["n_qn"], out=qn[:], in0=qv,
                           in1=st[:, 28:32].unsqueeze(2).to_broadcast([128, 4, 64]), op=ALU.mult)
                    pv4 = self.ps[4][:].bitcast(BF16)
                    qn2 = qn[:].rearrange("p h d -> p (h d)")
                    for c in range(2):
                        self.TR(["n_qn", "identb"], [("ps", 4)], out=pv4[:, 128 * c:128 * (c + 1)], in_=qn2[:, 128 * c:128 * (c + 1)],
                                identity=self.identb[:])
                    self.V("tensor_scalar", [("ps", 4), ("gcol", 0, 0), ("gcol", 0, 1)], [("n_qT", t)], out=qT[:, :, 128 * t:128 * (t + 1)],
                           in0=pv4[:, 0:256].rearrange("p (c n) -> p c n", c=2), scalar1=self.gcol[:, 0:1], scalar2=None, op0=ALU.mult)
                    for ki, c0 in ((0, 0), (1, 128)):
                        self.A([("ps", 3)], ["n_junk", ("n_kss", ki)], out=junk[:], in_=p3[:, c0:c0 + 64], func=AF.Square,
                               accum_out=st[:, 32 + ki:33 + ki])
                        self.rstd(st[:, 32 + ki:33 + ki], ("n_kss", ki), st[:, 34 + ki:35 + ki], ("n_kr", ki), st[:, 36 + ki:37 + ki],
                                  ("n_krs", ki), 64)
                        self.V("tensor_scalar", [("ps", 3), ("n_krs", ki)], [("n_kd", ki)], out=kd[:, ki, :, :],
                               in0=p3[:, c0:c0 + 64].unsqueeze(1).to_broadcast([128, 2, 64]), scalar1=st[:, 36 + ki:37 + ki], scalar2=None,
                               op0=ALU.mult)
                    pv5 = self.ps[5][:].bitcast(BF16)
                    for ki in range(2):
                        self.TR([("n_kd", ki), "identb"], [("ps", 5)], out=pv5[:, 128 * ki:128 * (ki + 1)],
                                in_=kd[:, ki, :, :].rearrange("p a d -> p (a d)"), identity=self.identb[:])
                    self.V("tensor_scalar", [("ps", 5), ("gcol", 1, 0), ("gcol", 1, 1)], [("n_ksT", t)], out=ksT[:, 128 * t:128 * (t + 1)],
                           in0=pv5[:, 0:128], scalar1=self.gcol[:, 1:2], scalar2=None, op0=ALU.mult)
                    self.V("tensor_scalar", [("ps", 5), ("gcol", 2, 0), ("gcol", 2, 1)], [("n_kwT", t)], out=kwT[:, 128 * t:128 * (t + 1)],
                           in0=pv5[:, 128:256], scalar1=self.gcol[:, 2:3], scalar2=None, op0=ALU.mult)
                    self.A([("ps", 3)], [("n_vs", t)], out=vsa[:, t, 0:64], in_=p3[:, 64:128], func=AF.Copy)
                    self.A([("ps", 3)], [("n_vw", t)], out=vwa[:, t, 0:64], in_=p3[:, 192:256], func=AF.Copy)
                    self.A([("ps", 3)], ["n_gtmp"], out=gtmp[:], in_=p3[:, 256:268], func=AF.Exp, scale=-1.0)
                    self.V("tensor_scalar", ["n_gtmp"], ["n_gtmp"], out=gtmp[:], in0=gtmp[:], scalar1=1.0, scalar2=None, op0=ALU.add)
                    self.V("reciprocal", ["n_gtmp"], [("n_g", t)], out=gates[:, t, :], in_=gtmp[:])
              self.barrier()
              if self.stop_after != (l, "nsa1"):
                with ExitStack() as es3:
                    sb3 = lambda n, shp, dt: self.sb(n, shp, dt, es3)
                    W1 = sb3("n_W1", [128, 16, 256], BF16)
                    BT = sb3("n_BT", [128, 16, 127], BF16)
                    w2c = sb3("n_w2c", [128, 2, 2, 64], BF16)
                    hdT = sb3("n_hdT", [128, 2, 2, 127], BF16)
                    ex = sb3("n_ex", [128, 127], F32)
                    kcd = sb3("n_kcd", [128, 2, 64], BF16)
                    for kv in range(2):
                        self.DMA("pool", [], [("n_w2c", kv)], "nw2%d" % kv, out=w2c[:, kv, :, :],
                                 in_=dr["cmp_w2"][l, kv].rearrange("(jc p) d -> p jc d", p=128))
                    allkv = [("n_kvcT", tb) for tb in range(4)]
                    for ih in range(2):
                        for kv in range(2):
                            self.DMA("pool", [], [("n_W1", kv)], "nw1%d" % kv, out=W1[64 * kv:64 * kv + 64, :, :],
                                     in_=dr["cmp_w1"][l, kv].rearrange("(i d) j -> d i j", d=64)[:, 16 * ih:16 * ih + 16, :])
                        for ii in range(16):
                            i = 16 * ih + ii
                            self.V("tensor_scalar", allkv + [("posT", 0), ("posT", 1)], [("n_BT", ii)], out=BT[:, ii, :],
                                   in0=kvcT[:, i:i + 2017:16], scalar1=self.posT[:, i:i + 1], scalar2=None, op0=ALU.add)
                        for kv in range(2):
                            for jc in range(2):
                                pb = 2 * kv + jc
                                for ii in range(16):
                                    i = 16 * ih + ii
                                    self.MM([("n_W1", kv), ("n_BT", ii)], [("ps", pb)], out=self.ps[pb][:, 0:127],
                                            lhsT=W1[64 * kv:64 * kv + 64, ii, 128 * jc:128 * (jc + 1)], rhs=BT[64 * kv:64 * kv + 64, ii, :],
                                            start=(i == 0), stop=(i == 31))
                    for kv in range(2):
                        for jc in range(2):
                            pb = 2 * kv + jc
                            self.A([("ps", pb)], ["n_ex"], out=ex[:], in_=self.ps[pb][:, 0:127], func=AF.Exp, scale=-1.0)
                            self.V("tensor_scalar", ["n_ex"], ["n_ex"], out=ex[:], in0=ex[:], scalar1=1.0, scalar2=None, op0=ALU.add)
                            self.V("reciprocal", ["n_ex"], ["n_ex2"], out=ex[:], in_=ex[:])
                            self.V("tensor_tensor", [("ps", pb), "n_ex2"], [("n_hdT", kv, jc)], out=hdT[:, kv, jc, :], in0=self.ps[pb][:, 0:127],
                                   in1=ex[:], op=ALU.mult)
                    for kv in range(2):
                        pb = 4 + kv
                        for jc in range(2):
                            self.MM([("n_hdT", kv, jc), ("n_w2c", kv)], [("ps", pb)], out=self.ps[pb][0:127, 0:64], lhsT=hdT[:, kv, jc, :],
                                    rhs=w2c[:, kv, jc, :], start=(jc == 0), stop=(jc == 1))
                    self.A([("ps", 4)], ["n_junk", "n_css"], out=junk[0:127, :], in_=self.ps[4][0:127, 0:64], func=AF.Square,
                           accum_out=st[0:127, 38:39])
                    self.rstd(st[0:127, 38:39], "n_css", st[0:127, 39:40], "n_cr", st[0:127, 40:41], "n_crs", 64)
                    self.V("memset", [], ["n_kcd"], ap=kcd[:], constant=0.0)
                    self.V("tensor_scalar", [("ps", 4), "n_crs", "n_kcd"], ["n_kcd"], out=kcd[0:127, :, :],
                           in0=self.ps[4][0:127, 0:64].unsqueeze(1).to_broadcast([127, 2, 64]), scalar1=st[0:127, 40:41], scalar2=None,
                           op0=ALU.mult)
                    pv6 = self.ps[6][:].bitcast(BF16)
                    self.TR(["n_kcd", "identb"], [("ps", 6)], out=pv6[:, 0:128], in_=kcd[:].rearrange("p a d -> p (a d)"),
                            identity=self.identb[:])
                    self.V("tensor_scalar", [("ps", 6), ("gcol", 3, 0), ("gcol", 3, 1)], ["n_kcT"], out=kcT[:], in0=pv6[:, 0:128],
                           scalar1=self.gcol[:, 3:4], scalar2=None, op0=ALU.mult)
                    self.V("memset", [], ["n_vc"], ap=vcs[:], constant=0.0)
                    self.A([("ps", 5), "n_vc"], ["n_vc"], out=vcs[0:127, :], in_=self.ps[5][0:127, 0:64], func=AF.Copy)
            self.barrier()
            if self.stop_after in ((l, "nsa1"), (l, "nsa2")):
                return
            Ycmb2 = [sb("n_Y%d" % i, [128, 4, 256], F32) for i in range(2)]
            nselT = [sb("n_nselT%d" % i, [128, 512], BF16) for i in range(2)]
            pc = sb("n_pc", [128, 4, 128], F32)
            dtc = sb("n_dtc", [128, 4, 128], F32)
            pcb = sb("n_pcb", [128, 4, 128], BF16)
            pcT = sb("n_pcT", [128, 4, 128], BF16)
            P4 = sb("n_P4", [128, 128], F32)
            sc = sb("n_sc", [128, 4, 32], F32)
            m8 = sb("n_m8", [128, 16], F32)
            nsel = sb("n_nsel", [128, 96], BF16)
            self.V("memset", [], ["n_nsel"], ap=nsel[:], constant=0.0)
            coef = sb("n_coef", [128, 8], F32)
            self.V("memset", [], ["n_pc"], ap=pc[:], constant=0.0)

            def cmp_stage(qb):
                Ycmb = Ycmb2[qb % 2]
                for j in range(4):
                    t = 4 * qb + j
                    for h in range(4):
                        p0 = 64 * (h % 2)
                        pbk = 6 + (h % 2)
                        self.MM([("n_qT", t), "n_kcT"], [("ps", pbk)], out=self.ps[pbk][:, 128 * (h // 2):128 * (h // 2) + 128],
                                lhsT=qT[p0:p0 + 64, h // 2, 128 * t:128 * (t + 1)], rhs=kcT[p0:p0 + 64, :], start=True, stop=True)
                    off = 120 - 8 * t
                    for par in range(2):
                        sv_ = self.ps[6 + par][:, 0:256].rearrange("p (h n) -> p h n", h=2)
                        self.V("scalar_tensor_tensor", [("ps", 6 + par), "n_tbl"], [("n_dtc", par)], out=dtc[:, par:4:2, :], in0=sv_, scalar=0.125,
                               in1=TcFull[:, par:4:2, off:off + 128], op0=ALU.mult, op1=ALU.add)
                    self.A([("n_dtc", 0), ("n_dtc", 1), "n_pc"], ["n_pc"], out=pc[:], in_=dtc[:], func=AF.Exp)
                    if CUT < 1:
                        continue
                    self.V("tensor_reduce", ["n_pc"], ["n_crs4"], out=st[:, 44:48], in_=pc[:], axis=AX.X, op=ALU.add)
                    self.V("tensor_scalar", ["n_crs4"], ["n_crs4b"], out=st[:, 48:52], in0=st[:, 44:48], scalar1=1e-30, scalar2=None,
                           op0=ALU.max)
                    self.V("reciprocal", ["n_crs4b"], ["n_cri"], out=st[:, 52:56], in_=st[:, 48:52])
                    self.V("tensor_tensor", ["n_pc", "n_cri"], ["n_pc"], out=pc[:], in0=pc[:],
                           in1=st[:, 52:56].unsqueeze(2).to_broadcast([128, 4, 128]), op=ALU.mult)
                    if CUT < 2:
                        continue
                    self.V("tensor_copy", ["n_pc"], ["n_pcb"], out=pcb[:], in_=pc[:])
                    pv7 = self.ps[2][:].bitcast(BF16)
                    for h in range(4):
                        self.TR(["n_pcb", "identb"], [("ps", 2)], out=pv7[:, 128 * h:128 * (h + 1)], in_=pcb[:, h, :],
                                identity=self.identb[:])
                    self.A([("ps", 2)], ["n_pcT"], out=pcT[:], in_=pv7[:, 0:512].rearrange("p (h n) -> p h n", h=4), func=AF.Copy)
                    for h in range(4):
                        self.MM(["n_pcT", "n_vc"], [("ps", 3)], out=self.ps[3][:, 64 * h:64 * (h + 1)], lhsT=pcT[:, h, :],
                                rhs=vcs[:, :], start=True, stop=True)
                    for h in range(4):
                        self.V("tensor_scalar", [("ps", 3), ("n_g", t)], [("n_Y", qb % 2, j, h)], out=Ycmb[:, j, 64 * h:64 * (h + 1)],
                               in0=self.ps[3][:, 64 * h:64 * (h + 1)], scalar1=gates[:, t, 3 * h:3 * h + 1], scalar2=None, op0=ALU.mult)
                    if CUT < 3:
                        continue
                    self.V("tensor_reduce", ["n_pc"], ["n_P4"], out=P4[:], in_=pc[:].rearrange("p h n -> p n h"), axis=AX.X, op=ALU.add)
                    P4v = P4[:].rearrange("p (j r) -> p j r", r=4)
                    self.V("tensor_reduce", ["n_P4"], [("n_sc", 0)], out=sc[:, 0, :], in_=P4v[:, :, 0:3], axis=AX.X, op=ALU.add)
                    self.V("scalar_tensor_tensor", ["n_P4", ("n_sc", 0)], [("n_sc", 1)], out=sc[:, 1, :], in0=P4v[:, :, 3], scalar=0.5,
                           in1=sc[:, 0, :], op0=ALU.mult, op1=ALU.add)
                    self.V("scalar_tensor_tensor", ["n_P4", ("n_sc", 1)], [("n_sc", 1)], out=sc[:, 1, 1:32], in0=P4v[:, 0:31, 3], scalar=0.5,
                           in1=sc[:, 1, 1:32], op0=ALU.mult, op1=ALU.add)
                    if CUT < 4:
                        continue
                    self.V("tensor_tensor", [("n_sc", 1), "n_vm"], [("n_sc", 2)], out=sc[:, 2, :], in0=sc[:, 1, :], in1=vm[:, t, :],
                           op=ALU.mult)
                    self.V("tensor_tensor", [("n_sc", 2), "n_ad"], [("n_sc", 2)], out=sc[:, 2, :], in0=sc[:, 2, :], in1=ad[:, t, :],
                           op=ALU.add)
                    if CUT < 5:
                        continue
                    self.V("max", [("n_sc", 2)], ["n_m8a"], out=m8[:, 0:8], in_=sc[:, 2, :])
                    self.V("match_replace", [("n_sc", 2), "n_m8a"], [("n_sc", 3)], out=sc[:, 3, :], in_to_replace=m8[:, 0:8],
                           in_values=sc[:, 2, :], imm_value=-2.0 * BIG)
                    self.V("max", [("n_sc", 3)], ["n_m8b"], out=m8[:, 8:16], in_=sc[:, 3, :])
                    if CUT < 6:
                        continue
                    for dup in range(2):
                        self.V("tensor_scalar", [("n_sc", 2), "n_m8b", "n_nsel"], ["n_nsel"], out=nsel[:, 64 * dup:64 * dup + 32], in0=sc[:, 2, :],
                               scalar1=m8[:, 15:16], scalar2=NEG, op0=ALU.is_lt, op1=ALU.mult)
                    if CUT < 7:
                        continue
                    self.TR(["n_nsel", "identb"], [("ps", 2)], out=pv7[0:96, 512:640], in_=nsel[:], identity=self.identb[:])
                    self.V("tensor_copy", [("ps", 2)], [("n_nselT", qb % 2, j)], out=nselT[qb % 2][0:96, 128 * j:128 * (j + 1)],
                           in_=pv7[0:96, 512:640])

            items = []
            g = 0
            for qb in range(4):
                for br in ("slc", "win"):
                    for h in range(4):
                        k0 = 0 if br == "slc" else max(0, 4 * qb - 4)
                        nk = 4 * qb + 4
                        for kt in range(k0, nk):
                            lo = max(kt, 4 * qb)
                            hi = 4 * qb + 3 if br == "slc" else min(kt + 4, 4 * qb + 3)
                            tiles = []
                            start, stop = {}, {}
                            for qt in range(lo, hi + 1):
                                j = qt - 4 * qb
                                if qt == kt:
                                    kind = (TD[:, h, :], ["n_tbl"])
                                elif qt == kt + 1:
                                    kind = (TO[:, h, :], ["n_tbl"])
                                elif br == "win" and qt == kt + 4:
                                    kind = (TW[:, h, :], ["n_tbl"])
                                else:
                                    kind = None
                                tiles.append((j, kind))
                                start[j] = (kt == (0 if br == "slc" else max(0, qt - 4)))
                                stop[j] = (kt == qt)
                            it = dict(qb=qb, h=h, kt=kt, g=g, br=br, last=(kt == nk - 1), gfirst=(kt == k0), tiles=tiles, start=start,
                                      stop=stop)
                            if br == "slc" and h == 0 and kt == 0:
                                it["pre"] = (lambda qb=qb: cmp_stage(qb))
                            items.append(it)
                        g += 1

            def score_fn(it, zb, c0, c1):
                qb, h, kt = it["qb"], it["h"], it["kt"]
                if "pre" in it:
                    it["pre"]()
                p0 = 64 * (h % 2)
                qres = [("n_qT", t) for t in range(4 * qb, 4 * qb + 4)]
                if it["br"] == "slc":
                    self.MM(qres + [("n_ksT", kt)], [("ps", zb)], out=self.ps[zb][:, c0:c1], lhsT=ksT[p0:p0 + 64, 128 * kt:128 * (kt + 1)],
                            rhs=qT[p0:p0 + 64, h // 2, 512 * qb + c0:512 * qb + c1], start=True, stop=False)
                    self.MM([("n_E", 0), ("n_E", 1)] + [("n_nselT", qb % 2, j) for j in range(4)], [("ps", zb)], out=self.ps[zb][:, c0:c1],
                            lhsT=E[p0:p0 + 32, 128 * kt:128 * (kt + 1)], rhs=nselT[qb % 2][p0:p0 + 32, c0:c1], start=False, stop=True)
                else:
                    self.MM(qres + [("n_kwT", kt)], [("ps", zb)], out=self.ps[zb][:, c0:c1], lhsT=kwT[p0:p0 + 64, 128 * kt:128 * (kt + 1)],
                            rhs=qT[p0:p0 + 64, h // 2, 512 * qb + c0:512 * qb + c1], start=True, stop=True)

            def v_fn(it):
                if it["br"] == "slc":
                    return vsa[:, it["kt"], :], [("n_vs", it["kt"])], 65
                return vwa[:, it["kt"], :], [("n_vw", it["kt"])], 65

            def finish(it, acc):
                qb, h = it["qb"], it["h"]
                Ycmb = Ycmb2[qb % 2]
                gi = 3 * h + (1 if it["br"] == "slc" else 2)
                av = self.ps[acc][:, 0:260].rearrange("p (j d) -> p j d", j=4)
                self.V("tensor_scalar", [("ps", acc)], ["n_rsum"], out=st[:, 60:64], in0=av[:, :, 64], scalar1=1e-30, scalar2=None,
                       op0=ALU.max)
                self.V("reciprocal", ["n_rsum"], ["n_rinv"], out=st[:, 56:60], in_=st[:, 60:64])
                self.V("tensor_tensor", ["n_rinv"] + [("n_g", t) for t in range(4 * qb, 4 * qb + 4)], ["n_coef"], out=coef[:, 0:4],
                       in0=st[:, 56:60], in1=gates[:, 4 * qb:4 * qb + 4, gi], op=ALU.mult)
                for j in range(4):
                    self.V("scalar_tensor_tensor", [("ps", acc), "n_coef", ("n_Y", qb % 2, j, h)], [("n_Y", qb % 2, j, h)], out=Ycmb[:, j, 64 * h:64 * (h + 1)],
                           in0=av[:, j, 0:64], scalar=coef[:, j:j + 1], in1=Ycmb[:, j, 64 * h:64 * (h + 1)], op0=ALU.mult, op1=ALU.add)
                if it["br"] == "win" and h == 3:
                    for j in range(4):
                        self.group_norm(Ycmb[:, j, :], [("n_Y", qb % 2, j, hh) for hh in range(4)], 1, 4 * qb + j, j % 2)

            if self.stop_after == (l, "nsa3"):
                for qb in range(4):
                    cmp_stage(qb)
                return
            self.attn_core(abufs, "n", 64, items, score_fn, None, 0.125, v_fn, finish, cbias=lambda it: self.c31[:, it["h"]:it["h"] + 1])
            self.dump("yn_nsa", self.Yn[:, :, 256:512], [128, NT, 256], [("Yn", t, 1) for t in range(NT)])

    def out_proj(self, l):
        dr = self.dram
        with ExitStack() as es:
            wo = self.sb("o_w", [128, 8, D], BF16, es)
            yT = [self.sb("o_yT%d" % i, [128, 8, 128], BF16, es) for i in range(2)]
            src = dr["w_out"][l].rearrange("(c p) n -> p c n", p=128)
            for q in range(4):
                self.DMA("pool", [], [("o_w", q)], "ow%d" % q, out=wo[:, 2 * q:2 * q + 2, :], in_=src[:, 2 * q:2 * q + 2, :])
            ong = self.sb("o_g", [128, 8], F32, es)
            onl = dr["out_norm_w"][l]
            self.DMA("sp", [], ["o_g"], "p3", out=ong[:], in_=bass.AP(tensor=onl.tensor, offset=onl.offset, ap=[[1, 128], [128, 8]]),
                     allow_slow_non_contiguous=True)
            for c in range(8):
                self.I("pool" if c % 2 else "dve", "tensor_scalar", [("o_w", c // 2), "o_g"], [("o_w", c // 2)], out=wo[:, c, :], in0=wo[:, c, :],
                       scalar1=ong[:, c:c + 1], scalar2=None, op0=ALU.mult)
            wres = [("o_w", q) for q in range(4)]
            for t in range(NT):
                b = t % 2
                pb = 6 + b
                pv = self.ps[pb][:].bitcast(BF16)
                for c in range(8):
                    self.TR([("Yn", t, c // 2), "identb"], [("ps", pb)], out=pv[:, 128 * c:128 * (c + 1)],
                            in_=self.Yn[:, t, 128 * c:128 * (c + 1)], identity=self.identb[:])
                self.A([("ps", pb)], [("o_yT", b)], out=yT[b][:], in_=pv.rearrange("p (c n) -> p c n", c=8), func=AF.Copy)
                for hf in range(2):
                    ob = 2 * b + hf
                    for c in range(8):
                        self.MM([("o_yT", b)] + wres, [("ps", ob)], out=self.ps[ob][:, :], lhsT=yT[b][:, c, :],
                                rhs=wo[:, c, 512 * hf:512 * (hf + 1)], start=(c == 0), stop=(c == 7))
                    self.V("tensor_tensor", [("ps", ob), ("X", t)], [("X", t)], out=self.X[:, t, 512 * hf:512 * (hf + 1)],
                           in0=self.ps[ob][:, :], in1=self.X[:, t, 512 * hf:512 * (hf + 1)], op=ALU.add)
            self.dump("xmid", self.X[:], [128, NT, D], [("X", t) for t in range(NT)])

    def ffn(self, l):
        dr = self.dram
        hT = self.hT
        uT = self.Yn
        with ExitStack() as es:
            w2h = self.sb("f_w2", [128, 16, D], BF16, es)
            w1c = [self.sb("f_w1_%d" % i, [128, 8, 128], BF16, es) for i in range(3)]
            self.f_r = [self.sb("f_r%d" % i, [128, 512], F32, es) for i in range(4)]
            w1src = dr["ffn_w1"][l].rearrange("(c p) f -> p c f", p=128)
            w2src = dr["ffn_w2"][l].rearrange("(c p) n -> p c n", p=128)
            k = 0
            for fh in range(2):
                for q in range(4):
                    self.DMA("pool", [], [("f_w2", q)], "fw2%d" % q, out=w2h[:, 4 * q:4 * q + 4, :],
                             in_=w2src[:, 16 * fh + 4 * q:16 * fh + 4 * q + 4, :])
                for th in range(2):
                    for fc in range(16):
                        wb = k % 3
                        k += 1
                        f0 = (16 * fh + fc) * 128
                        self.DMA("pool", [], [("f_w1", wb)], "fw1%d" % wb, out=w1c[wb][:], in_=w1src[:, :, f0:f0 + 128])
                        for tb2 in range(2):
                            tb = 2 * th + tb2
                            pb = (2 * fc + tb2) % 4
                            for dc in range(8):
                                self.MM(self.hT_res(tb) + [("f_w1", wb)], [("ps", pb)], out=self.ps[pb][:, :], lhsT=w1c[wb][:, dc, :],
                                        rhs=hT[:, dc, 512 * tb:512 * (tb + 1)], start=(dc == 0), stop=(dc == 7))
                            dst = uT[:, fc, 512 * tb2:512 * (tb2 + 1)]
                            self.A([("ps", pb)], [("f_r", pb)], out=self.f_r[pb][:], in_=self.ps[pb][:, :], func=AF.Relu)
                            self.V("tensor_tensor", [("f_r", pb)], [("f_u", fc, tb2)], out=dst, in0=self.f_r[pb][:], in1=self.f_r[pb][:],
                                   op=ALU.mult)
                    for tt in range(8):
                        t = 8 * th + tt
                        for hf in range(2):
                            ob = 4 + (2 * tt + hf) % 4
                            for fc in range(16):
                                self.MM([("f_u", fc, tt // 4), ("f_w2", fc // 4)], [("ps", ob)], out=self.ps[ob][:, :],
                                        lhsT=uT[:, fc, 128 * tt:128 * (tt + 1)], rhs=w2h[:, fc, 512 * hf:512 * (hf + 1)],
                                        start=(fc == 0), stop=(fc == 15))
                            self.V("tensor_tensor", [("ps", ob), ("X", t)], [("X", t)], out=self.X[:, t, 512 * hf:512 * (hf + 1)],
                                   in0=self.ps[ob][:, :], in1=self.X[:, t, 512 * hf:512 * (hf + 1)], op=ALU.add)


_CACHE = {}


def run(inputs, n_cores=8, stop_after=None, dbg=(), trace=False):
    key = (stop_after, tuple(dbg))
    b = Builder(stop_after=stop_after, dbg=dbg)
    nc = b.build()
    consts = make_consts()
    shared = {k: np.ascontiguousarray(np.asarray(inputs[k], dtype=np.float32)) for k in WEIGHT_SHAPES}
    if stop_after is not None:
        shared["ffn_w1"] = np.zeros((NL, 128, 128), np.float32)
        shared["ffn_w2"] = np.zeros((NL, 128, 128), np.float32)
    shared.update(consts)
    x = np.asarray(inputs["x"], dtype=np.float32)
    in_maps = []
    for i in range(n_cores):
        m = dict(shared)
        m["x"] = np.ascontiguousarray(x[i])
        in_maps.append(m)
    res = run_bass_kernel_spmd(nc, in_maps, core_ids=list(range(n_cores)), trace=trace)
    return res, b


def kernel(**inputs):
    res, b = run(inputs, n_cores=8)
    out = np.stack([np.asarray(r["out"], dtype=np.float32) for r in res.results], axis=0)
    return out
```

```python
import math
import os
from contextlib import ExitStack

import numpy as np
import concourse.bass as bass
import concourse.mybir as mybir
from concourse.bass_utils import run_bass_kernel_spmd

F32 = mybir.dt.float32
BF16 = mybir.dt.bfloat16
AF = mybir.ActivationFunctionType
ALU = mybir.AluOpType
AX = mybir.AxisListType

S = 2048
D = 1024
NT = 16
NL = 2
NEG = -30000.0
EPS = 1e-6
BIG = 1.0e9
IN_COLS = 2540
C_A, C_N, C_M, C_S = 0, 768, 1420, 1772

ENG_NAMES = ("pe", "act", "dve", "pool", "sp")
CUT = int(os.environ.get("NSA_CUT", "99"))


class Op:
    __slots__ = ("eng", "fn", "deps", "is_dma", "sem", "val", "idx", "signal", "snap", "waits", "cidx")

    def __init__(self, eng, fn, is_dma, sem):
        self.eng = eng
        self.fn = fn
        self.is_dma = is_dma
        self.sem = sem
        self.deps = set()
        self.val = 0
        self.signal = is_dma
        self.snap = None
        self.waits = []
        self.cidx = -1


class Prog:
    def __init__(self, nc):
        self.nc = nc
        self.ops = []
        self.last_w = {}
        self.readers = {}
        self.ncomp = {e: 0 for e in ENG_NAMES}

    def op(self, eng, fn, reads=(), writes=(), dma_sem=None, track=True):
        o = Op(eng, fn, dma_sem is not None, dma_sem if dma_sem is not None else eng)
        o.idx = len(self.ops)
        if not o.is_dma:
            o.cidx = self.ncomp[eng]
            self.ncomp[eng] += 1
        for r in reads:
            w = self.last_w.get(r)
            if w is not None:
                o.deps.add(w)
        for r in writes:
            w = self.last_w.get(r)
            if w is not None:
                o.deps.add(w)
            for x in self.readers.get(r, ()):
                o.deps.add(x)
        if track:
            for r in reads:
                self.readers.setdefault(r, []).append(o.idx)
            for r in writes:
                self.last_w[r] = o.idx
                self.readers[r] = []
        o.deps.discard(o.idx)
        self.ops.append(o)
        return o

    def _needs_wait(self, o, d):
        if d.is_dma or o.is_dma:
            return True
        if d.eng != o.eng:
            return True
        if o.eng == "pe":
            return False
        return (o.cidx - d.cidx) <= 1

    def finalize(self, sems):
        ops = self.ops
        for o in ops:
            for di in o.deps:
                d = ops[di]
                if self._needs_wait(o, d):
                    d.signal = True
        cnt = {}
        for o in ops:
            if o.signal:
                inc = 16 if o.is_dma else 1
                cnt[o.sem] = cnt.get(o.sem, 0) + inc
                o.val = cnt[o.sem]
        known = {e: {} for e in ENG_NAMES}
        for o in ops:
            k = known[o.eng]
            need = {}
            for di in o.deps:
                d = ops[di]
                if not self._needs_wait(o, d):
                    continue
                if k.get(d.sem, 0) >= d.val:
                    continue
                if need.get(d.sem, (0, None))[0] < d.val:
                    need[d.sem] = (d.val, d)
            for s, (v, d) in need.items():
                if k.get(s, 0) >= v:
                    continue
                o.waits.append((s, v))
                k[s] = v
                if d.snap:
                    for s2, v2 in d.snap.items():
                        if k.get(s2, 0) < v2:
                            k[s2] = v2
            if o.signal:
                o.snap = dict(k)
                if not o.is_dma:
                    o.snap[o.sem] = o.val
        per = {e: [] for e in ENG_NAMES}
        for o in ops:
            per[o.eng].append(o)

        def emit(engobj, lst):
            for o in lst:
                for s, v in o.waits:
                    engobj.wait_ge(sems[s], v)
                ins = o.fn(engobj)
                if o.signal:
                    assert ins is not None
                    ins.then_inc(sems[o.sem], 16 if o.is_dma else 1)

        with self.nc.Block() as block:
            @block.tensor
            def _(e):
                emit(e, per["pe"])

            @block.scalar
            def _(e):
                emit(e, per["act"])

            @block.vector
            def _(e):
                emit(e, per["dve"])

            @block.gpsimd
            def _(e):
                emit(e, per["pool"])

            @block.sync
            def _(e):
                emit(e, per["sp"])

    def sem_names(self):
        s = set(ENG_NAMES)
        for o in self.ops:
            s.add(o.sem)
        return sorted(s, key=str)


def _bucket(d):
    d = np.maximum(d, 0)
    df = np.maximum(d, 1).astype(np.float32)
    large = 16 + (np.log(df / np.float32(16)) / np.float32(math.log(8.0)) * np.float32(16)).astype(np.int32)
    large = np.minimum(large, 31)
    return np.where(d < 16, d, large)


def make_consts():
    c = {}
    c["c_ident"] = np.eye(128, dtype=np.float32)
    inv = (1.0 / (np.float32(10000.0) ** (np.arange(0, 32, 2, dtype=np.float32) / np.float32(32)))).astype(np.float32)
    ang = np.arange(S, dtype=np.float32)[:, None] * inv[None, :]
    cs = np.cos(ang).astype(np.float32).reshape(NT, 128, 16).transpose(1, 0, 2)
    sn = np.sin(ang).astype(np.float32).reshape(NT, 128, 16).transpose(1, 0, 2)
    c["c_cos"] = np.ascontiguousarray(cs)
    c["c_sin"] = np.ascontiguousarray(sn)
    oh = np.zeros((32, 1152), np.float32)
    negrow = np.zeros((1, 1152), np.float32)
    for i in range(256):
        u = i - 127
        if 0 <= u <= 127:
            oh[_bucket(np.array(u)), i] = 1.0
        else:
            negrow[0, i] = NEG
        if -127 <= u <= 127:
            oh[_bucket(np.array(128 + u)), 256 + i] = 1.0
        if u < 0:
            oh[31, 512 + i] = 1.0
        else:
            negrow[0, 512 + i] = NEG
    for i in range(384):
        dist = 240 - i
        if dist >= 0:
            oh[_bucket(np.array(dist)), 768 + i] = 1.0
        else:
            negrow[0, 768 + i] = NEG
    c["c_oh"] = oh
    c["c_negrow"] = negrow
    e = np.zeros((32, S), np.float32)
    e[np.arange(S) // 64, np.arange(S)] = 1.0
    c["c_e"] = e
    t = np.arange(S)
    blk = np.arange(32)[None, :]
    tb = (t // 64)[:, None]
    valid = blk * 64 <= t[:, None]
    forced = (blk == 0) | (blk == tb) | (blk == tb - 1)
    vm = (valid & ~forced).astype(np.float32)
    ad = np.where(forced, BIG, np.where(valid, 0.0, -BIG)).astype(np.float32)
    c["c_vm"] = np.ascontiguousarray(vm.reshape(NT, 128, 32).transpose(1, 0, 2))
    c["c_ad"] = np.ascontiguousarray(ad.reshape(NT, 128, 32).transpose(1, 0, 2))
    j = np.arange(128)
    c["c_tri"] = (j[:, None] >= j[None, :]).astype(np.float32)
    c["c_ones"] = np.ones((128, 128), np.float32)
    c["c_mcausal"] = np.where(j[None, :] >= j[:, None], 0.0, NEG).astype(np.float32)
    c["c_mstrict"] = (j[None, :] > j[:, None]).astype(np.float32)
    return c


CONST_SHAPES = {
    "c_ident": [128, 128], "c_cos": [128, NT, 16], "c_sin": [128, NT, 16], "c_oh": [32, 1152],
    "c_negrow": [1, 1152], "c_e": [32, S], "c_vm": [128, NT, 32], "c_ad": [128, NT, 32],
    "c_tri": [128, 128], "c_ones": [128, 128], "c_mcausal": [128, 128], "c_mstrict": [128, 128],
}

WEIGHT_SHAPES = {
    "rel_bias": [32, 4], "norm1_w": [NL, D], "w_in": [NL, D, IN_COLS], "conv_w": [NL, 3, 256], "conv_b": [NL, 256],
    "nsa_q_norm": [NL, 64], "nsa_k_norm": [NL, 3, 64], "cmp_pos": [NL, 2, 32, 64], "cmp_w1": [NL, 2, 2048, 256],
    "cmp_w2": [NL, 2, 256, 64], "mla_q_a_norm": [NL, 192], "mla_kv_norm": [NL, 128], "mla_wq_b": [NL, 192, 384],
    "mla_wkv_b": [NL, 128, 512], "mla_q_norm": [NL, 96], "mla_k_norm": [NL, 96], "out_norm_w": [NL, D],
    "w_out": [NL, D, D], "norm2_w": [NL, D], "ffn_w1": [NL, D, 4096], "ffn_w2": [NL, 4096, D],
}


class Builder:
    def __init__(self, stop_after=None, dbg=()):
        self.stop_after = stop_after
        self.dbg = set(dbg)
        self.nc = bass.Bass("TRN2", target_bir_lowering=False)
        self.P = Prog(self.nc)
        self.es = ExitStack()
        self.dram = {}
        self.dbg_outs = {}
        self.nsem = 0

    def sb(self, name, shape, dt, es=None):
        self.nsb = getattr(self, "nsb", 0) + 1
        return (es or self.es).enter_context(self.nc.sbuf_tensor("%s_u%d" % (name, self.nsb), shape, dt))

    def I(self, eng, method, reads, writes, **kw):
        return self.P.op(eng, lambda e, kw=kw, m=method: getattr(e, m)(**kw), reads, writes)

    def V(self, method, reads, writes, **kw):
        return self.I("dve", method, reads, writes, **kw)

    def G(self, method, reads, writes, **kw):
        return self.I("pool", method, reads, writes, **kw)

    def A(self, reads, writes, **kw):
        return self.I("act", "activation", reads, writes, **kw)

    def MM(self, reads, writes, **kw):
        return self.I("pe", "matmul", reads, writes, **kw)

    def TR(self, reads, writes, **kw):
        return self.I("pe", "transpose", reads, writes, **kw)

    def DMA(self, q, reads, writes, sem, **kw):
        return self.P.op(q, lambda e, kw=kw: e.dma_start(**kw), reads, writes, dma_sem=sem)

    def rstd(self, src, src_res, tmp, tmp_res, dst, dst_res, n):
        self.A([src_res, "epsc"], [tmp_res], out=tmp, in_=src, func=AF.Sqrt, scale=1.0 / n, bias=self.epsc[0:src.shape[0], :])
        self.V("reciprocal", [tmp_res], [dst_res], out=dst, in_=tmp)

    def barrier(self):
        P = self.P
        allres = list(P.last_w.keys() | P.readers.keys())
        self.MM([], allres + ["BAR"], out=self.ps[7][0:1, 0:8], lhsT=self.identb[0:1, 0:1], rhs=self.identb[0:1, 0:8],
                start=True, stop=True)
        self.A([], allres + ["BAR"], out=self.bar_s[0:1, 0:8], in_=self.bar_s[0:1, 8:16], func=AF.Copy)
        self.V("memset", [], allres + ["BAR"], ap=self.bar_s[0:1, 16:24], constant=0.0)
        self.G("memset", [], allres + ["BAR"], ap=self.bar_s[0:1, 24:32], constant=0.0)
        self.MM(["BAR"], ["BARpe"], out=self.ps[7][0:1, 0:8], lhsT=self.identb[0:1, 0:1], rhs=self.identb[0:1, 0:8],
                start=True, stop=True)
        self.A(["BAR"], ["BARact"], out=self.bar_s[0:1, 0:8], in_=self.bar_s[0:1, 8:16], func=AF.Copy)
        self.V("memset", ["BAR"], ["BARdve"], ap=self.bar_s[0:1, 16:24], constant=0.0)
        self.G("memset", ["BAR"], ["BARpool"], ap=self.bar_s[0:1, 24:32], constant=0.0)
        self.P.op("sp", lambda e: None, ["BAR"], [], track=False)

    def dump(self, name, ap, shape, reads):
        if name not in self.dbg:
            return
        t = self.nc.dram_tensor("dbg_" + name, list(shape), ap.dtype, kind="ExternalOutput").ap()
        self.dbg_outs[name] = t
        self.DMA("sp", reads, [("dbgout", name)], "dbg_" + name, out=t, in_=ap)

    def declare(self):
        nc = self.nc
        dr = self.dram
        dr["x"] = nc.dram_tensor("x", [S, D], F32, kind="ExternalInput").ap()
        for k, shp in WEIGHT_SHAPES.items():
            if self.stop_after is not None and k in ("ffn_w1", "ffn_w2"):
                shp = [NL, 128, 128]
            dr[k] = nc.dram_tensor(k, shp, F32, kind="ExternalInput").ap()
        for k, shp in CONST_SHAPES.items():
            dr[k] = nc.dram_tensor(k, shp, F32, kind="ExternalInput").ap()
        dr["out"] = nc.dram_tensor("out", [S, D], F32, kind="ExternalOutput").ap()
        dr["rs"] = nc.dram_tensor("rs_scratch", [4, 128, 1152], F32, kind="Internal").ap()
        dr["tbl"] = nc.dram_tensor("tbl_scratch", [128, 2528], F32, kind="Internal").ap()

    def build(self):
        with self.es:
            self.declare()
            self._build()
            names = self.P.sem_names()
            sems = {n: self.es.enter_context(self.nc.semaphore("s%d" % i)) for i, n in enumerate(names)}
            self.nsem = len(names)
            self.P.finalize(sems)
        return self.nc

    def _build(self):
        nc, dr = self.nc, self.dram
        sb = self.sb
        self.ps = [self.es.enter_context(nc.psum_tensor("ps%d" % i, [128, 512], F32)) for i in range(8)]
        self.X = sb("X", [128, NT, D], F32)
        self.hT = sb("hT", [128, 8, S], BF16)
        self.Yn = sb("Yn", [128, NT, D], BF16)
        self.identb = sb("identb", [128, 128], BF16)
        self.identf = sb("identf", [128, 128], F32)
        self.bar_s = sb("bar_s", [1, 32], F32)
        self.epsc = sb("epsc", [128, 1], F32)
        self.onec = sb("onec", [128, 1], F32)
        self.ntri = sb("ntri", [128, 128], BF16)
        self.nones = sb("nones", [128, 128], BF16)
        self.zrow = sb("zrow", [1, 512], BF16)
        self.V("memset", [], ["zrow"], ap=self.zrow[:], constant=0.0)
        self.V("memset", [], ["onec"], ap=self.onec[:], constant=1.0)
        self.V("memset", [], ["epsc"], ap=self.epsc[:], constant=EPS)
        self.tri = sb("tri", [128, 128], BF16)
        self.ones = sb("ones", [128, 128], BF16)
        self.mcausal = sb("mcausal", [128, 128], F32)
        self.mstrict = sb("mstrict", [128, 128], F32)
        self.c31 = sb("c31", [128, 4], F32)
        self.gcol = sb("gcol", [128, 8], F32)
        self.cw = sb("cw", [128, 6], F32)
        self.cb = sb("cb", [128, 2], F32)
        self.posT = sb("posT", [128, 32], F32)
        self.stat = sb("stat", [128, 64], F32)

        D_ = self.DMA
        D_("sp", [], ["identf"], "c1", out=self.identf[:], in_=dr["c_ident"][:])
        D_("pool", [], ["identb"], "c2", out=self.identb[:], in_=dr["c_ident"][:])
        D_("pool", [], ["tri"], "c5", out=self.tri[:], in_=dr["c_tri"][:])
        D_("pool", [], ["ones"], "c6", out=self.ones[:], in_=dr["c_ones"][:])
        D_("sp", [], ["mcausal"], "c7", out=self.mcausal[:], in_=dr["c_mcausal"][:])
        D_("sp", [], ["mstrict"], "c8", out=self.mstrict[:], in_=dr["c_mstrict"][:])
        D_("sp", [], ["c31"], "c12", out=self.c31[:], in_=dr["rel_bias"][31:32, :].partition_broadcast(128))
        for i in range(4):
            D_("sp", [], [("X", t) for t in range(4 * i, 4 * i + 4)], "x%d" % i, out=self.X[:, 4 * i:4 * i + 4, :],
               in_=dr["x"][512 * i:512 * (i + 1), :].rearrange("(t p) d -> p t d", p=128))
        self.V("tensor_scalar", ["tri"], ["ntri"], out=self.ntri[:], in0=self.tri[:], scalar1=-1.0, scalar2=None, op0=ALU.mult)
        self.V("tensor_scalar", ["ones"], ["nones"], out=self.nones[:], in0=self.ones[:], scalar1=-1.0, scalar2=None, op0=ALU.mult)
        self.build_bias_tables()
        for l in range(NL):
            self.layer(l)
            if self.stop_after is not None and self.stop_after[0] == l:
                break
        outs = []
        for t in range(NT):
            D_("sp", [("X", t)], [("out", t)], "out", out=dr["out"][128 * t:128 * (t + 1), :], in_=self.X[:, t, :])
            outs.append(("out", t))
        outs += [("dbgout", n) for n in self.dbg_outs]
        self.P.op("sp", lambda e: None, outs, [], track=False)

    def build_bias_tables(self):
        nc, dr = self.nc, self.dram
        with ExitStack() as es:
            rb = self.sb("rb", [32, 4], F32, es)
            rbm = self.sb("rbm", [32, 4, 128], F32, es)
            oh = self.sb("oh", [32, 1152], F32, es)
            negt = self.sb("negt", [128, 1152], F32, es)
            rrow = self.sb("rrow", [128, 4, 1152], F32, es)
            band = self.sb("band", [128, 4, 16], F32, es)
            tbl = self.sb("tbl_b", [128, 2528], F32, es)
            self.TD = tbl[:, 0:512].rearrange("p (h n) -> p h n", h=4)
            self.TO = tbl[:, 512:1024].rearrange("p (h n) -> p h n", h=4)
            self.TW = tbl[:, 1024:1536].rearrange("p (h n) -> p h n", h=4)
            self.TcFull = tbl[:, 1536:2528].rearrange("p (h n) -> p h n", h=4)
            self.DMA("sp", [], ["rb"], "b1", out=rb[:], in_=dr["rel_bias"][:])
            self.DMA("sp", [], ["oh"], "b2", out=oh[:], in_=dr["c_oh"][:])
            self.DMA("sp", [], ["negt"], "b3", out=negt[:], in_=dr["c_negrow"][:].partition_broadcast(128))
            self.V("tensor_copy", ["rb"], ["rbm"], out=rbm[:], in_=rb[:].unsqueeze(2).to_broadcast([32, 4, 128]))
            for h in range(4):
                for j in range(3):
                    pb = (h * 3 + j) % 2
                    self.MM(["rbm", "oh"], [("ps", pb)], out=self.ps[pb][:, 0:384], lhsT=rbm[:, h, :],
                            rhs=oh[:, 384 * j:384 * (j + 1)], start=True, stop=True)
                    self.V("tensor_tensor", [("ps", pb), "negt"], [("rrow", h)], out=rrow[:, h, 384 * j:384 * (j + 1)],
                           in0=self.ps[pb][:, 0:384], in1=negt[:, 384 * j:384 * (j + 1)], op=ALU.add)
                self.DMA("sp", [("rrow", h)], [("rs", h)], "b4%d" % h, out=dr["rs"][h], in_=rrow[:, h, :])
            rst = dr["rs"].tensor
            for h in range(4):
                base = h * 128 * 1152
                for nm, tl, off in (("TD", self.TD, 127), ("TO", self.TO, 256 + 127), ("TW", self.TW, 512 + 127)):
                    src = bass.AP(tensor=rst, offset=base + off, ap=[[1151, 128], [1, 128]])
                    self.DMA("sp", [("rs", h)], [(nm, h)], "b5%s%d" % (nm, h), out=tl[:, h, :], in_=src)
                src = bass.AP(tensor=rst, offset=base + 768 + 127, ap=[[1151, 128], [16, 16]])
                self.DMA("sp", [("rs", h)], [("band", h)], "b6%d" % h, out=band[:, h, :], in_=src,
                         allow_slow_non_contiguous=True)
            self.V("memset", [], ["TcFull"], ap=self.TcFull, constant=NEG)
            for h in range(4):
                self.V("tensor_scalar", ["c31", "TcFull"], ["TcFull"], out=self.TcFull[:, h, 0:111],
                       in0=self.TcFull[:, h, 0:111], scalar1=0.0, scalar2=self.c31[:, h:h + 1], op0=ALU.mult, op1=ALU.add)
                self.V("tensor_copy", [("band", h), "TcFull"], ["TcFull"], out=self.TcFull[:, h, 111:127], in_=band[:, h, :])
            allt = ["TcFull"] + [(nm, h) for nm in ("TD", "TO", "TW") for h in range(4)]
            self.dump("tbl", tbl[:], [128, 2528], allt)
            self.DMA("sp", allt, ["tbl_dram"], "b7", out=dr["tbl"][:], in_=tbl[:])
            self.barrier()

    def load_w(self, l, es, c0, ncols):
        w = self.sb("wsl", [128, 8, ncols], BF16, es)
        src = self.dram["w_in"][l, :, c0:c0 + ncols].rearrange("(c p) n -> p c n", p=128)
        for half in range(2):
            self.DMA("pool", [], [("wsl", half)], "wb%d" % half,
                     out=w[:, 4 * half:4 * half + 4, :], in_=src[:, 4 * half:4 * half + 4, :])
        return w, [("wsl", 0), ("wsl", 1)]

    def layer(self, l):
        dr = self.dram
        D_ = self.DMA

        def col(ap1d, n):
            return bass.AP(tensor=ap1d.tensor, offset=ap1d.offset, ap=[[1, n], [1, 1]])
        k = 0
        for ci, src, n in ((0, dr["nsa_q_norm"][l], 64), (1, dr["nsa_k_norm"][l, 1], 64), (2, dr["nsa_k_norm"][l, 2], 64),
                           (3, dr["nsa_k_norm"][l, 0], 64)):
            for half in range(2):
                D_("sp", [], [("gcol", ci, half)], "g%d%d" % (ci, half), out=self.gcol[64 * half:64 * half + 64, ci:ci + 1],
                   in_=col(src, 64))
        D_("sp", [], [("gcol", 4, 0)], "g40", out=self.gcol[0:96, 4:5], in_=col(dr["mla_q_norm"][l], 96))
        D_("sp", [], [("gcol", 5, 0)], "g50", out=self.gcol[0:96, 5:6], in_=col(dr["mla_k_norm"][l], 96))
        cwl = dr["conv_w"][l]
        D_("sp", [], ["cw"], "p6", out=self.cw[:], in_=bass.AP(tensor=cwl.tensor, offset=cwl.offset, ap=[[1, 128], [128, 6]]),
           allow_slow_non_contiguous=True)
        D_("sp", [], ["cb"], "p7", out=self.cb[:], in_=dr["conv_b"][l].rearrange("(c p) -> p c", p=128),
           allow_slow_non_contiguous=True)
        for kv in range(2):
            D_("sp", [], [("posT", kv)], "p8%d" % kv, out=self.posT[64 * kv:64 * kv + 64, :],
               in_=dr["cmp_pos"][l, kv].rearrange("i d -> d i"), allow_slow_non_contiguous=True)
        self.norm_transpose(l, "norm1_w", "n1t")
        self.mixer_a(l)
        self.barrier()
        if self.stop_after == (l, "a"):
            return
        self.mixer_sb(l)
        self.barrier()
        if self.stop_after == (l, "sb"):
            return
        self.mixer_mla(l)
        self.barrier()
        if self.stop_after == (l, "mla"):
            return
        self.mixer_nsa(l)
        self.barrier()
        if self.stop_after in ((l, "nsa"), (l, "nsa1"), (l, "nsa2"), (l, "nsa3")):
            return
        self.out_proj(l)
        self.barrier()
        if self.stop_after == (l, "out"):
            return
        self.norm_transpose(l, "norm2_w", "n2t")
        self.ffn(l)
        self.barrier()

    def norm_transpose(self, l, wname, gname):
        X, hT, st = self.X, self.hT, self.stat
        with ExitStack() as es:
            gt = self.sb("nt_g", [128, D], F32, es)
            self.DMA("sp", [], [gname], "p1", out=gt[:], in_=self.dram[wname][l:l + 1, :].partition_broadcast(128))
            junk = self.sb("nt_junk", [128, D], BF16, es)
            hn = [self.sb("nt_hn%d" % i, [128, D], BF16, es) for i in range(2)]
            for t in range(NT):
                b = t % 2
                self.A([("X", t)], ["nt_junk", ("nt_ssq", b)], out=junk[:], in_=X[:, t, :], func=AF.Square,
                       accum_out=st[:, b:b + 1])
                self.rstd(st[:, b:b + 1], ("nt_ssq", b), st[:, 2 + b:3 + b], ("nt_r", b), st[:, 4 + b:5 + b], ("nt_rs", b), D)
                self.V("scalar_tensor_tensor", [("X", t), ("nt_rs", b), gname], [("nt_hn", b)], out=hn[b][:], in0=X[:, t, :],
                       scalar=st[:, 4 + b:5 + b], in1=gt[:], op0=ALU.mult, op1=ALU.mult)
                pb = 6 + b
                pv = self.ps[pb][:].bitcast(BF16)
                for c in range(8):
                    self.TR([("nt_hn", b), "identb"], [("ps", pb)], out=pv[:, 128 * c:128 * (c + 1)],
                            in_=hn[b][:, 128 * c:128 * (c + 1)], identity=self.identb[:])
                eng = "act" if t % 2 == 0 else "dve"
                outap = hT[:, :, 128 * t:128 * (t + 1)]
                inap = pv.rearrange("p (c n) -> p c n", c=8)
                if eng == "act":
                    self.A([("ps", pb)], [("hT", t)], out=outap, in_=inap, func=AF.Copy)
                else:
                    self.V("tensor_copy", [("ps", pb)], [("hT", t)], out=outap, in_=inap)
        self.barrier()

    def hT_res(self, tb):
        return [("hT", t) for t in range(4 * tb, 4 * tb + 4)]

    def group_norm(self, yap, yres, g, t, slot):
        st = self.stat
        o = 8 + 4 * slot
        junk = self.gn_junk[slot]
        self.A(yres, [("gn_junk", slot), ("gn_ssq", slot)], out=junk[:], in_=yap, func=AF.Square,
               accum_out=st[:, o:o + 1])
        self.rstd(st[:, o:o + 1], ("gn_ssq", slot), st[:, o + 1:o + 2], ("gn_r", slot), st[:, o + 2:o + 3], ("gn_rs", slot), 256)
        self.V("tensor_scalar", yres + [("gn_rs", slot)], [("Yn", t, g)],
               out=self.Yn[:, t, 256 * g:256 * (g + 1)], in0=yap, scalar1=st[:, o + 2:o + 3], scalar2=None, op0=ALU.mult)

    def mixer_a(self, l):
        hT = self.hT
        with ExitStack() as es:
            w, wr = self.load_w(l, es, C_A, 768)
            self.gn_junk = [self.sb("gn_junk%d" % i, [128, 256], BF16, es) for i in range(2)]
            v = self.sb("a_v", [128, 2, S + 2], F32, es)
            cs = [self.sb("a_cs%d" % i, [128, 512], F32, es) for i in range(2)]
            y = [self.sb("a_y%d" % i, [128, 512], F32, es) for i in range(2)]
            ya = [self.sb("a_ya%d" % i, [128, 2, 512], F32, es) for i in range(2)]
            self.V("memset", [], [("a_v", 0), ("a_v", 1)], ap=v[:, :, 0:2], constant=0.0)
            for tb in range(4):
                hres = self.hT_res(tb)
                yb = tb % 2
                for c in range(2):
                    pbn = [0 + 3 * c, 1 + 3 * c, 2 + 3 * c]
                    for gi, co in enumerate((0, 256, 512)):
                        pb = pbn[gi]
                        for dc in range(8):
                            self.MM(hres + wr, [("ps", pb)], out=self.ps[pb][:, :],
                                    lhsT=w[:, dc, co + 128 * c:co + 128 * (c + 1)], rhs=hT[:, dc, 512 * tb:512 * (tb + 1)],
                                    start=(dc == 0), stop=(dc == 7))
                    pbB, pbC, pbU = pbn
                    self.A([("ps", pbC)], [("a_cs", c)], out=cs[c][:], in_=self.ps[pbC][:], func=AF.Copy)
                    vs = v[:, c, 2 + 512 * tb:2 + 512 * (tb + 1)]
                    self.V("tensor_tensor", [("ps", pbU), ("a_cs", c)], [("a_v", c)], out=vs, in0=self.ps[pbU][:], in1=cs[c][:],
                           op=ALU.mult)
                    self.V("tensor_scalar", [("a_v", c), "cw", "cb"], [("a_y", c)], out=y[c][:], in0=vs,
                           scalar1=self.cw[:, 4 + c:5 + c], scalar2=self.cb[:, c:c + 1], op0=ALU.mult, op1=ALU.add)
                    self.V("scalar_tensor_tensor", [("a_v", c), "cw", ("a_y", c)], [("a_y", c)], out=y[c][:],
                           in0=v[:, c, 1 + 512 * tb:1 + 512 * (tb + 1)], scalar=self.cw[:, 2 + c:3 + c], in1=y[c][:],
                           op0=ALU.mult, op1=ALU.add)
                    self.V("scalar_tensor_tensor", [("a_v", c), "cw", ("a_y", c)], [("a_y", c)], out=y[c][:],
                           in0=v[:, c, 512 * tb:512 * (tb + 1)], scalar=self.cw[:, c:c + 1], in1=y[c][:],
                           op0=ALU.mult, op1=ALU.add)
                    self.V("tensor_tensor", [("ps", pbB), ("a_y", c)], [("a_ya", yb, c)], out=ya[yb][:, c, :],
                           in0=self.ps[pbB][:], in1=y[c][:], op=ALU.mult)
                for j in range(4):
                    t = 4 * tb + j
                    pb = 6 + (j % 2)
                    for c in range(2):
                        self.TR([("a_ya", yb, c), "identf"], [("ps", pb)], out=self.ps[pb][:, 128 * c:128 * (c + 1)],
                                in_=ya[yb][:, c, 128 * j:128 * (j + 1)], identity=self.identf[:])
                    self.group_norm(self.ps[pb][:, 0:256], [("ps", pb)], 0, t, j % 2)
            self.dump("yn_a", self.Yn[:, :, 0:256], [128, NT, 256], [("Yn", t, 0) for t in range(NT)])

    def pipeline(self, items, stages):
        n = len(items)
        ns = len(stages)
        for step in range(n + ns - 1):
            for si, st in enumerate(stages):
                i = step - si
                if 0 <= i < n:
                    st(i, items[i])

    def proj_feat(self, w, wr, col0, tb, pb):
        for dc in range(8):
            self.MM(self.hT_res(tb) + wr, [("ps", pb)], out=self.ps[pb][:, :], lhsT=w[:, dc, col0:col0 + 128],
                    rhs=self.hT[:, dc, 512 * tb:512 * (tb + 1)], start=(dc == 0), stop=(dc == 7))

    def proj_tok(self, w, wr, col0, ncol, t, pb):
        for dc in range(8):
            self.MM([("hT", t)] + wr, [("ps", pb)], out=self.ps[pb][:, 0:ncol], lhsT=self.hT[:, dc, 128 * t:128 * (t + 1)],
                    rhs=w[:, dc, col0:col0 + ncol], start=(dc == 0), stop=(dc == 7))

    def mixer_sb(self, l):
        with ExitStack() as es:
            sb = lambda n, shp, dt: self.sb(n, shp, dt, es)
            w, wr = self.load_w(l, es, C_S, 768)
            self.gn_junk = [sb("gn_junk%d" % i, [128, 256], BF16) for i in range(2)]
            sqT = sb("s_qT", [128, 2, S], BF16)
            skT = sb("s_kT", [128, 2, S], BF16)
            sv = sb("s_v", [128, NT, 256], BF16)
            NB = 3
            e_t = [sb("s_e%d" % i, [128, 512], F32) for i in range(NB)]
            LTb = [sb("s_L%d" % i, [128, 512], BF16) for i in range(NB)]
            aT = [sb("s_a%d" % i, [128, 512], BF16) for i in range(NB)]
            Sf = sb("s_Sf", [128, 512], F32)
            Sb = [sb("s_Sb%d" % i, [128, 512], BF16) for i in range(2)]
            Ysb = sb("s_Y", [128, 4, 256], F32)
            k = 0
            for tb in range(4):
                for gi in range(2):
                    for c in range(2):
                        pb = k % 4
                        k += 1
                        self.proj_feat(w, wr, gi * 256 + c * 128, tb, pb)
                        dst = (sqT if gi == 0 else skT)[:, c, 512 * tb:512 * (tb + 1)]
                        dres = ("s_qT" if gi == 0 else "s_kT", c, tb)
                        if gi == 0:
                            self.V("tensor_scalar", [("ps", pb)], [dres], out=dst, in0=self.ps[pb][:], scalar1=0.125, scalar2=None, op0=ALU.mult)
                        else:
                            self.A([("ps", pb)], [dres], out=dst, in_=self.ps[pb][:], func=AF.Copy)
            for t in range(NT):
                pb = 4 + t % 2
                self.proj_tok(w, wr, 512, 256, t, pb)
                if t % 2 == 0:
                    self.A([("ps", pb)], [("s_v", t)], out=sv[:, t, :], in_=self.ps[pb][:, 0:256], func=AF.Copy)
                else:
                    self.V("tensor_copy", [("ps", pb)], [("s_v", t)], out=sv[:, t, :], in_=self.ps[pb][:, 0:256])
            items = []
            g = 0
            for qb in range(4):
                for h in range(4):
                    nk = 4 * qb + 4
                    for i, kt in enumerate(range(nk - 1, -1, -1)):
                        items.append(dict(qb=qb, h=h, kt=kt, first=(i == 0), last=(kt == 0), g=g))
                    g += 1

            def geom(it):
                qb, kt = it["qb"], it["kt"]
                f = max(kt, 4 * qb)
                c0 = (f - 4 * qb) * 128
                return f, c0, kt >= 4 * qb

            def st1(i, it):
                qb, h, kt = it["qb"], it["h"], it["kt"]
                f, c0, diag = geom(it)
                c, p0 = h // 2, 64 * (h % 2)
                b = i % NB
                zb = i % 4
                qres = [("s_qT", c, qb)]
                self.MM(qres + [("s_kT", c, kt // 4)], [("ps", zb)], out=self.ps[zb][:, c0:512],
                        lhsT=skT[p0:p0 + 64, c, 128 * kt:128 * (kt + 1)], rhs=sqT[p0:p0 + 64, c, 512 * qb + c0:512 * (qb + 1)],
                        start=True, stop=True)
                self.A([("ps", zb)], [("s_e", b)], out=e_t[b][:, c0:512], in_=self.ps[zb][:, c0:512], func=AF.Exp)
                self.A([("s_e", b), "onec"], [("s_L", b)], out=LTb[b][:, c0:512], in_=e_t[b][:, c0:512], func=AF.Ln, bias=self.onec[:, :])
                if diag:
                    self.V("tensor_tensor", [("s_L", b), "mstrict"], [("s_L", b)], out=LTb[b][:, c0:c0 + 128],
                           in0=LTb[b][:, c0:c0 + 128], in1=self.mstrict[:], op=ALU.mult)

            def st2(i, it):
                f, c0, diag = geom(it)
                b = i % NB
                zb = i % 4
                first = it["first"]
                self.MM([("s_L", b), "ntri"], [("ps", zb)], out=self.ps[zb][:, c0:512], lhsT=self.ntri[:], rhs=LTb[b][:, c0:512],
                        start=False, stop=first)
                if not first:
                    self.MM([("s_Sb", i % 2), "nones"], [("ps", zb)], out=self.ps[zb][:, c0:512], lhsT=self.nones[:],
                            rhs=Sb[i % 2][:, c0:512], start=False, stop=True)
                self.A([("ps", zb)], [("s_a", b)], out=aT[b][:, c0:512], in_=self.ps[zb][:, c0:512], func=AF.Exp)
                if diag:
                    self.V("tensor_tensor", [("s_a", b), "mstrict"], [("s_a", b)], out=aT[b][:, c0:c0 + 128],
                           in0=aT[b][:, c0:c0 + 128], in1=self.mstrict[:], op=ALU.mult)
                if not it["last"]:
                    if first:
                        self.V("memset", [], ["s_Sf"], ap=Sf[:], constant=0.0)
                    self.V("tensor_tensor", ["s_Sf", ("s_L", b)], ["s_Sf"], out=Sf[:, c0:512], in0=Sf[:, c0:512],
                           in1=LTb[b][:, c0:512], op=ALU.add)
                    self.V("tensor_copy", ["s_Sf"], [("s_Sb", (i + 1) % 2)], out=Sb[(i + 1) % 2][:], in_=Sf[:])

            def st3(i, it):
                qb, h, kt = it["qb"], it["h"], it["kt"]
                f, c0, diag = geom(it)
                b = i % NB
                acc = 4 + it["g"] % 2
                if it["first"]:
                    self.MM(["zrow"], [("ps", acc)], out=self.ps[acc][:, 0:256], lhsT=self.zrow[0:1, 0:128], rhs=self.zrow[0:1, 0:256],
                            start=True, stop=False)
                for j in range(f - 4 * qb, 4):
                    self.MM([("s_a", b), ("s_v", kt)], [("ps", acc)], out=self.ps[acc][:, 64 * j:64 * (j + 1)],
                            lhsT=aT[b][:, 128 * j:128 * (j + 1)], rhs=sv[:, kt, 64 * h:64 * (h + 1)],
                            start=False, stop=(kt == 0 and j == 3))
                if it["last"]:
                    self.A([("ps", acc)], [("s_Y", j, h) for j in range(4)], out=Ysb[:, :, 64 * h:64 * (h + 1)],
                           in_=self.ps[acc][:, 0:256].rearrange("p (j d) -> p j d", j=4), func=AF.Copy)
                    if h == 3:
                        for j in range(4):
                            self.group_norm(Ysb[:, j, :], [("s_Y", j, hh) for hh in range(4)], 3, 4 * qb + j, j % 2)

            self.pipeline(items, [st1, st2, st3])
            self.dump("yn_sb", self.Yn[:, :, 768:1024], [128, NT, 256], [("Yn", t, 3) for t in range(NT)])

    def attn_bufs(self, es, tag):
        pT = [self.sb("%s_p%d" % (tag, i), [128, 512], BF16, es) for i in range(3)]
        dtmp = [self.sb("%s_d%d" % (tag, i), [128, 512], F32, es) for i in range(2)]
        return pT, dtmp

    def attn_core(self, bufs, tag, nheads_K, items, score_fn, bias_fn, scale, v_fn, finish_fn, cbias=None):
        NB = 3
        pT, dtmp = bufs

        def st1(i, it):
            zb = i % 2
            b = i % 2
            tiles = it["tiles"]
            c0 = 128 * tiles[0][0]
            c1 = 128 * (tiles[-1][0] + 1)
            score_fn(it, zb, c0, c1)
            for j, kind in tiles:
                if kind is not None:
                    bap, bres = kind
                    self.V("scalar_tensor_tensor", [("ps", zb)] + bres, [(tag + "_d", b, j)], out=dtmp[b][:, 128 * j:128 * (j + 1)],
                           in0=self.ps[zb][:, 128 * j:128 * (j + 1)], scalar=scale, in1=bap, op0=ALU.mult, op1=ALU.add)

        def st2(i, it):
            zb = i % 2
            b = i % NB
            b2 = i % 2
            tiles = it["tiles"]
            run = []
            runs = []
            for j, kind in tiles:
                if kind is None:
                    run.append(j)
                else:
                    if run:
                        runs.append(run)
                        run = []
                    self.A([(tag + "_d", b2, j)], [(tag + "_p", b, j)], out=pT[b][:, 128 * j:128 * (j + 1)],
                           in_=dtmp[b2][:, 128 * j:128 * (j + 1)], func=AF.Exp)
            if run:
                runs.append(run)
            for r in runs:
                a0, a1 = 128 * r[0], 128 * (r[-1] + 1)
                kw = dict(out=pT[b][:, a0:a1], in_=self.ps[zb][:, a0:a1], func=AF.Exp, scale=scale)
                rr = [("ps", zb)]
                if cbias is not None:
                    kw["bias"] = cbias(it)
                    rr.append("c31")
                self.A(rr, [(tag + "_p", b, j) for j in r], **kw)

        def st3(i, it):
            b = i % NB
            acc = 4 + it["g"] % 2
            vap, vres, vw = v_fn(it)
            if it["gfirst"]:
                self.MM(["zrow"], [("ps", acc)], out=self.ps[acc][:, 0:4 * vw], lhsT=self.zrow[0:1, 0:128], rhs=self.zrow[0:1, 0:4 * vw],
                        start=True, stop=False)
            for j, kind in it["tiles"]:
                self.MM([(tag + "_p", b, j)] + vres, [("ps", acc)], out=self.ps[acc][:, vw * j:vw * (j + 1)],
                        lhsT=pT[b][:, 128 * j:128 * (j + 1)], rhs=vap, start=False, stop=(it["last"] and j == it["tiles"][-1][0]))
            if it["last"]:
                finish_fn(it, acc)

        self.pipeline(items, [st1, st2, st3])

    def rope(self, x1, x2, t, nh, tmp, res_in, res_tmp):
        shp = [128, nh, 16]
        cb = self.cos[:, t, :].unsqueeze(1).to_broadcast(shp)
        sb_ = self.sin[:, t, :].unsqueeze(1).to_broadcast(shp)
        t1, t2, t3, t4 = [tmp[:, i, 0:nh, :] for i in range(4)]
        rd = res_in + ["cos", "sin"]
        self.V("tensor_tensor", rd, [res_tmp + "1"], out=t1, in0=x1, in1=cb, op=ALU.mult)
        self.V("tensor_tensor", rd, [res_tmp + "2"], out=t2, in0=x2, in1=sb_, op=ALU.mult)
        self.V("tensor_tensor", rd, [res_tmp + "3"], out=t3, in0=x1, in1=sb_, op=ALU.mult)
        self.V("tensor_tensor", rd, [res_tmp + "4"], out=t4, in0=x2, in1=cb, op=ALU.mult)
        self.V("tensor_tensor", [res_tmp + "1", res_tmp + "2"], res_in, out=x1, in0=t1, in1=t2, op=ALU.subtract)
        self.V("tensor_tensor", [res_tmp + "3", res_tmp + "4"], res_in, out=x2, in0=t3, in1=t4, op=ALU.add)

    def head_norm_T(self, src, src_res, nh, hd, t, dstT, dst_res, gidx, sq, qn, stc, pb, tag):
        st = self.stat
        self.V("tensor_tensor", src_res, [tag + "_sq"], out=sq[:, 0:nh, 0:hd], in0=src, in1=src, op=ALU.mult)
        self.V("tensor_reduce", [tag + "_sq"], [tag + "_ss"], out=st[:, stc:stc + nh], in_=sq[:, 0:nh, 0:hd], axis=AX.X, op=ALU.add)
        self.rstd(st[:, stc:stc + nh], tag + "_ss", st[:, stc + 4:stc + 4 + nh], tag + "_r", st[:, stc + 8:stc + 8 + nh], tag + "_rs", hd)
        self.V("tensor_tensor", src_res + [tag + "_rs"], [tag + "_n"], out=qn[:, 0:nh, 0:hd], in0=src,
               in1=st[:, stc + 8:stc + 8 + nh].unsqueeze(2).to_broadcast([128, nh, hd]), op=ALU.mult)
        pv = self.ps[pb][:].bitcast(BF16)
        for h in range(nh):
            self.TR([tag + "_n", "identb"], [("ps", pb)], out=pv[0:hd, 128 * h:128 * (h + 1)], in_=qn[:, h, 0:hd],
                    identity=self.identb[:])
        self.V("tensor_scalar", [("ps", pb), ("gcol", gidx, 0)], [dst_res], out=dstT[0:hd, :, 128 * t:128 * (t + 1)],
               in0=pv[0:hd, 0:128 * nh].rearrange("p (h n) -> p h n", h=nh), scalar1=self.gcol[0:hd, gidx:gidx + 1], scalar2=None,
               op0=ALU.mult)

    def mixer_mla(self, l):
        dr = self.dram
        st = self.stat
        with ExitStack() as es:
            sb = lambda n, shp, dt: self.sb(n, shp, dt, es)
            self.gn_junk = [sb("gn_junk%d" % i, [128, 256], BF16) for i in range(2)]
            abufs = self.attn_bufs(es, "m")
            wqb = sb("m_wqb", [128, 2, 384], BF16)
            wkvb = sb("m_wkvb", [128, 512], BF16)
            self.DMA("pool", [], ["m_wqb0"], "mw0", out=wqb[:, 0, :], in_=dr["mla_wq_b"][l, 0:128, :])
            self.DMA("pool", [], ["m_wqb1"], "mw1", out=wqb[0:64, 1, :], in_=dr["mla_wq_b"][l, 128:192, :])
            self.DMA("pool", [], ["m_wkvb"], "mw2", out=wkvb[:], in_=dr["mla_wkv_b"][l])
            qmT = sb("m_qT", [128, 4, S], BF16)
            kmT = sb("m_kT", [128, 4, S], BF16)
            vma = sb("m_v", [128, NT, 4, 65], BF16)
            Ysb = sb("m_Y", [128, 4, 256], F32)
            self.V("memset", [], [("m_v", t) for t in range(NT)], ap=vma[:, :, :, 64:65], constant=1.0)
            with ExitStack() as es2:
                sb2 = lambda n, shp, dt: self.sb(n, shp, dt, es2)
                w, wr = self.load_w(l, es2, C_M, 352)
                self.cos = sb2("cos", [128, NT, 16], F32)
                self.sin = sb2("sin", [128, NT, 16], F32)
                self.qat = sb2("qat", [128, 192], F32)
                self.kvt = sb2("kvt", [128, 128], F32)
                self.DMA("sp", [], ["cos"], "c3", out=self.cos[:], in_=dr["c_cos"][:])
                self.DMA("sp", [], ["sin"], "c4", out=self.sin[:], in_=dr["c_sin"][:])
                self.DMA("sp", [], ["qat"], "p4", out=self.qat[:], in_=dr["mla_q_a_norm"][l:l + 1, :].partition_broadcast(128))
                self.DMA("sp", [], ["kvt"], "p5", out=self.kvt[:], in_=dr["mla_kv_norm"][l:l + 1, :].partition_broadcast(128))
                junk = sb2("m_junk", [128, 192], BF16)
                cqn = sb2("m_cqn", [128, 192], BF16)
                ckvn = sb2("m_ckvn", [128, 128], BF16)
                krs = sb2("m_krs", [128, 1, 32], F32)
                cT = sb2("m_cT", [128, 3, 128], BF16)
                qs = sb2("m_qs", [128, 4, 96], F32)
                ks_ = sb2("m_ks", [128, 4, 96], F32)
                sq = sb2("m_sq", [128, 4, 96], F32)
                qn = sb2("m_qn", [128, 4, 96], BF16)
                kn = sb2("m_kn", [128, 4, 96], BF16)
                rtmp = sb2("m_rt", [128, 4, 4, 16], F32)
                for t in range(NT):
                    self.proj_tok(w, wr, 0, 352, t, 0)
                    pm = self.ps[0]
                    self.A([("ps", 0)], ["m_junk", "m_ssq0"], out=junk[:, 0:192], in_=pm[:, 0:192], func=AF.Square,
                           accum_out=st[:, 20:21])
                    self.A([("ps", 0)], ["m_junk", "m_ssq1"], out=junk[:, 0:128], in_=pm[:, 192:320], func=AF.Square,
                           accum_out=st[:, 21:22])
                    self.rstd(st[:, 20:21], "m_ssq0", st[:, 22:23], "m_r0", st[:, 24:25], "m_rs0", 192)
                    self.rstd(st[:, 21:22], "m_ssq1", st[:, 23:24], "m_r1", st[:, 25:26], "m_rs1", 128)
                    self.V("scalar_tensor_tensor", [("ps", 0), "m_rs0", "qat"], ["m_cqn"], out=cqn[:], in0=pm[:, 0:192],
                           scalar=st[:, 24:25], in1=self.qat[:], op0=ALU.mult, op1=ALU.mult)
                    self.V("scalar_tensor_tensor", [("ps", 0), "m_rs1", "kvt"], ["m_ckvn"], out=ckvn[:], in0=pm[:, 192:320],
                           scalar=st[:, 25:26], in1=self.kvt[:], op0=ALU.mult, op1=ALU.mult)
                    self.A([("ps", 0)], ["m_krs"], out=krs[:, 0, :], in_=pm[:, 320:352], func=AF.Copy)
                    pv1 = self.ps[1][:].bitcast(BF16)
                    self.TR(["m_cqn", "identb"], [("ps", 1)], out=pv1[:, 0:128], in_=cqn[:, 0:128], identity=self.identb[:])
                    self.TR(["m_cqn", "identb"], [("ps", 1)], out=pv1[0:64, 128:256], in_=cqn[:, 128:192], identity=self.identb[:])
                    self.TR(["m_ckvn", "identb"], [("ps", 1)], out=pv1[:, 256:384], in_=ckvn[:], identity=self.identb[:])
                    self.A([("ps", 1)], ["m_cT"], out=cT[:], in_=pv1[:, 0:384].rearrange("p (c n) -> p c n", c=3), func=AF.Copy)
                    self.MM(["m_cT", "m_wqb0"], [("ps", 2)], out=self.ps[2][:, 0:384], lhsT=cT[:, 0, :], rhs=wqb[:, 0, :],
                            start=True, stop=False)
                    self.MM(["m_cT", "m_wqb1"], [("ps", 2)], out=self.ps[2][:, 0:384], lhsT=cT[0:64, 1, :], rhs=wqb[0:64, 1, :],
                            start=False, stop=True)
                    self.MM(["m_cT", "m_wkvb"], [("ps", 3)], out=self.ps[3][:, 0:512], lhsT=cT[:, 2, :], rhs=wkvb[:],
                            start=True, stop=True)
                    self.A([("ps", 2)], ["m_qs"], out=qs[:], in_=self.ps[2][:, 0:384].rearrange("p (h d) -> p h d", h=4), func=AF.Copy)
                    self.rope(qs[:, :, 64:80], qs[:, :, 80:96], t, 4, rtmp, ["m_qs"], "m_rt")
                    self.head_norm_T(qs[:], ["m_qs"], 4, 96, t, qmT, ("m_qT", t), 4, sq, qn, 28, 4, "m_q")
                    kvv = self.ps[3][:, 0:512].rearrange("p (h d) -> p h d", h=4)
                    self.rope(krs[:, :, 0:16], krs[:, :, 16:32], t, 1, rtmp, ["m_krs"], "m_rk")
                    self.V("tensor_copy", [("ps", 3)], ["m_ksA"], out=ks_[:, :, 0:64], in_=kvv[:, :, 0:64])
                    self.V("tensor_copy", ["m_krs"], ["m_ksB"], out=ks_[:, :, 64:96], in_=krs[:, 0:1, :].to_broadcast([128, 4, 32]))
                    self.head_norm_T(ks_[:], ["m_ksA", "m_ksB"], 4, 96, t, kmT, ("m_kT", t), 5, sq, kn, 44, 5, "m_k")
                    self.A([("ps", 3)], [("m_v", t)], out=vma[:, t, :, 0:64], in_=kvv[:, :, 64:128], func=AF.Copy)
            self.dump("m_qT", qmT[0:96, :, :], [96, 4, S], [("m_qT", t) for t in range(NT)])
            self.dump("m_kT", kmT[0:96, :, :], [96, 4, S], [("m_kT", t) for t in range(NT)])
            scale = 96 ** -0.5
            items = []
            g = 0
            for qb in range(4):
                for h in range(4):
                    nk = 4 * qb + 4
                    for kt in range(nk):
                        f = max(kt, 4 * qb)
                        tiles = [(j, ((self.mcausal[:], ["mcausal"]) if 4 * qb + j == kt else None)) for j in range(f - 4 * qb, 4)]
                        items.append(dict(qb=qb, h=h, kt=kt, g=g, last=(kt == nk - 1), gfirst=(kt == 0), tiles=tiles,
                                          start={j: kt == 0 for j in range(4)}, stop={j: kt == 4 * qb + j for j in range(4)}))
                    g += 1

            def score_fn(it, zb, c0, c1):
                qb, h, kt = it["qb"], it["h"], it["kt"]
                self.MM([("m_qT", t) for t in range(4 * qb, 4 * qb + 4)] + [("m_kT", kt)], [("ps", zb)], out=self.ps[zb][:, c0:c1],
                        lhsT=kmT[0:96, h, 128 * kt:128 * (kt + 1)], rhs=qmT[0:96, h, 512 * qb + c0:512 * qb + c1],
                        start=True, stop=True)

            def v_fn(it):
                return vma[:, it["kt"], it["h"], :], [("m_v", it["kt"])], 65

            def finish(it, acc):
                qb, h = it["qb"], it["h"]
                av = self.ps[acc][:, 0:260].rearrange("p (j d) -> p j d", j=4)
                self.V("tensor_scalar", [("ps", acc)], ["m_rsum"], out=st[:, 60:64], in0=av[:, :, 64], scalar1=1e-30, scalar2=None,
                       op0=ALU.max)
                self.V("reciprocal", ["m_rsum"], ["m_rinv"], out=st[:, 56:60], in_=st[:, 60:64])
                self.V("tensor_tensor", [("ps", acc), "m_rinv"], [("m_Y", j, h) for j in range(4)], out=Ysb[:, :, 64 * h:64 * (h + 1)],
                       in0=av[:, :, 0:64], in1=st[:, 56:60].unsqueeze(2).to_broadcast([128, 4, 64]), op=ALU.mult)
                if h == 3:
                    for j in range(4):
                        self.group_norm(Ysb[:, j, :], [("m_Y", j, hh) for hh in range(4)], 2, 4 * qb + j, j % 2)

            self.attn_core(abufs, "m", 96, items, score_fn, None, scale, v_fn, finish)
            self.dump("yn_mla", self.Yn[:, :, 512:768], [128, NT, 256], [("Yn", t, 2) for t in range(NT)])

    def mixer_nsa(self, l):
        dr = self.dram
        st = self.stat
        with ExitStack() as es:
            sb = lambda n, shp, dt: self.sb(n, shp, dt, es)
            self.gn_junk = [sb("gn_junk%d" % i, [128, 256], BF16) for i in range(2)]
            abufs = self.attn_bufs(es, "n")
            tbl = sb("n_tbl", [128, 2528], F32)
            self.DMA("sp", ["tbl_dram"], ["n_tbl"], "nt1", out=tbl[:], in_=dr["tbl"][:])
            TD = tbl[:, 0:512].rearrange("p (h n) -> p h n", h=4)
            TO = tbl[:, 512:1024].rearrange("p (h n) -> p h n", h=4)
            TW = tbl[:, 1024:1536].rearrange("p (h n) -> p h n", h=4)
            TcFull = tbl[:, 1536:2528].rearrange("p (h n) -> p h n", h=4)
            E = sb("n_E", [128, S], BF16)
            vm = sb("n_vm", [128, NT, 32], F32)
            ad = sb("n_ad", [128, NT, 32], F32)
            self.DMA("pool", [], [("n_E", 0)], "nt2", out=E[0:32, :], in_=dr["c_e"][:])
            self.DMA("pool", [], [("n_E", 1)], "nt5", out=E[64:96, :], in_=dr["c_e"][:])
            self.DMA("sp", [], ["n_vm"], "nt3", out=vm[:], in_=dr["c_vm"][:])
            self.DMA("sp", [], ["n_ad"], "nt4", out=ad[:], in_=dr["c_ad"][:])
            qT = sb("n_qT", [128, 2, S], BF16)
            ksT = sb("n_ksT", [128, S], BF16)
            kwT = sb("n_kwT", [128, S], BF16)
            vsa = sb("n_vs", [128, NT, 65], BF16)
            vwa = sb("n_vw", [128, NT, 65], BF16)
            gates = sb("n_g", [128, NT, 12], F32)
            kcT = sb("n_kcT", [128, 128], BF16)
            vcs = sb("n_vc", [128, 64], BF16)
            self.V("memset", [], [("n_vs", t) for t in range(NT)], ap=vsa[:, :, 64:65], constant=1.0)
            self.V("memset", [], [("n_vw", t) for t in range(NT)], ap=vwa[:, :, 64:65], constant=1.0)
            with ExitStack() as es_kv:
              kvcT = self.sb("n_kvcT", [128, S], F32, es_kv)
              junk = self.sb("n_junk", [128, 64], BF16, es_kv)
              with ExitStack() as es2:
                sb2 = lambda n, shp, dt: self.sb(n, shp, dt, es2)
                w, wr = self.load_w(l, es2, C_N, 652)
                sqq = sb2("n_sqq", [128, 4, 64], F32)
                qn = sb2("n_qn", [128, 4, 64], BF16)
                kd = sb2("n_kd", [128, 2, 2, 64], BF16)
                gtmp = sb2("n_gtmp", [128, 12], F32)
                for tb in range(4):
                    pb = tb % 2
                    self.proj_feat(w, wr, 256, tb, pb)
                    self.A([("ps", pb)], [("n_kvcT", tb)], out=kvcT[:, 512 * tb:512 * (tb + 1)], in_=self.ps[pb][:], func=AF.Copy)
                for t in range(NT):
                    self.proj_tok(w, wr, 0, 256, t, 2)
                    self.proj_tok(w, wr, 384, 268, t, 3)
                    p2, p3 = self.ps[2], self.ps[3]
                    qv = p2[:, 0:256].rearrange("p (h d) -> p h d", h=4)
                    self.A([("ps", 2)], ["n_sqq"], out=sqq[:], in_=qv, func=AF.Square)
                    self.V("tensor_reduce", ["n_sqq"], ["n_qss"], out=st[:, 20:24], in_=sqq[:], axis=AX.X, op=ALU.add)
                    self.rstd(st[:, 20:24], "n_qss", st[:, 24:28], "n_qr", st[:, 28:32], "n_qrs", 64)
                    self.V("tensor_tensor", [("ps", 2), "n_qrs"], ["n_qn"], out=qn[:], in0=qv,
                           in1=st[:, 28:32].unsqueeze(2).to_broadcast([128, 4, 64]), op=ALU.mult)
                    pv4 = self.ps[4][:].bitcast(BF16)
                    qn2 = qn[:].rearrange("p h d -> p (h d)")
                    for c in range(2):
                        self.TR(["n_qn", "identb"], [("ps", 4)], out=pv4[:, 128 * c:128 * (c + 1)], in_=qn2[:, 128 * c:128 * (c + 1)],
                                identity=self.identb[:])
                    self.V("tensor_scalar", [("ps", 4), ("gcol", 0, 0), ("gcol", 0, 1)], [("n_qT", t)], out=qT[:, :, 128 * t:128 * (t + 1)],
                           in0=pv4[:, 0:256].rearrange("p (c n) -> p c n", c=2), scalar1=self.gcol[:, 0:1], scalar2=None, op0=ALU.mult)
                    for ki, c0 in ((0, 0), (1, 128)):
                        self.A([("ps", 3)], ["n_junk", ("n_kss", ki)], out=junk[:], in_=p3[:, c0:c0 + 64], func=AF.Square,
                               accum_out=st[:, 32 + ki:33 + ki])
                        self.rstd(st[:, 32 + ki:33 + ki], ("n_kss", ki), st[:, 34 + ki:35 + ki], ("n_kr", ki), st[:, 36 + ki:37 + ki],
                                  ("n_krs", ki), 64)
                        self.V("tensor_scalar", [("ps", 3), ("n_krs", ki)], [("n_kd", ki)], out=kd[:, ki, :, :],
                               in0=p3[:, c0:c0 + 64].unsqueeze(1).to_broadcast([128, 2, 64]), scalar1=st[:, 36 + ki:37 + ki], scalar2=None,
                               op0=ALU.mult)
                    pv5 = self.ps[5][:].bitcast(BF16)
                    for ki in range(2):
                        self.TR([("n_kd", ki), "identb"], [("ps", 5)], out=pv5[:, 128 * ki:128 * (ki + 1)],
                                in_=kd[:, ki, :, :].rearrange("p a d -> p (a d)"), identity=self.identb[:])
                    self.V("tensor_scalar", [("ps", 5), ("gcol", 1, 0), ("gcol", 1, 1)], [("n_ksT", t)], out=ksT[:, 128 * t:128 * (t + 1)],
                           in0=pv5[:, 0:128], scalar1=self.gcol[:, 1:2], scalar2=None, op0=ALU.mult)
                    self.V("tensor_scalar", [("ps", 5), ("gcol", 2, 0), ("gcol", 2, 1)], [("n_kwT", t)], out=kwT[:, 128 * t:128 * (t + 1)],
                           in0=pv5[:, 128:256], scalar1=self.gcol[:, 2:3], scalar2=None, op0=ALU.mult)
                    self.A([("ps", 3)], [("n_vs", t)], out=vsa[:, t, 0:64], in_=p3[:, 64:128], func=AF.Copy)
                    self.A([("ps", 3)], [("n_vw", t)], out=vwa[:, t, 0:64], in_=p3[:, 192:256], func=AF.Copy)
                    self.A([("ps", 3)], ["n_gtmp"], out=gtmp[:], in_=p3[:, 256:268], func=AF.Exp, scale=-1.0)
                    self.V("tensor_scalar", ["n_gtmp"], ["n_gtmp"], out=gtmp[:], in0=gtmp[:], scalar1=1.0, scalar2=None, op0=ALU.add)
                    self.V("reciprocal", ["n_gtmp"], [("n_g", t)], out=gates[:, t, :], in_=gtmp[:])
              self.barrier()
              if self.stop_after != (l, "nsa1"):
                with ExitStack() as es3:
                    sb3 = lambda n, shp, dt: self.sb(n, shp, dt, es3)
                    W1 = sb3("n_W1", [128, 16, 256], BF16)
                    BT = sb3("n_BT", [128, 16, 127], BF16)
                    w2c = sb3("n_w2c", [128, 2, 2, 64], BF16)
                    hdT = sb3("n_hdT", [128, 2, 2, 127], BF16)
                    ex = sb3("n_ex", [128, 127], F32)
                    kcd = sb3("n_kcd", [128, 2, 64], BF16)
                    for kv in range(2):
                        self.DMA("pool", [], [("n_w2c", kv)], "nw2%d" % kv, out=w2c[:, kv, :, :],
                                 in_=dr["cmp_w2"][l, kv].rearrange("(jc p) d -> p jc d", p=128))
                    allkv = [("n_kvcT", tb) for tb in range(4)]
                    for ih in range(2):
                        for kv in range(2):
                            self.DMA("pool", [], [("n_W1", kv)], "nw1%d" % kv, out=W1[64 * kv:64 * kv + 64, :, :],
                                     in_=dr["cmp_w1"][l, kv].rearrange("(i d) j -> d i j", d=64)[:, 16 * ih:16 * ih + 16, :])
                        for ii in range(16):
                            i = 16 * ih + ii
                            self.V("tensor_scalar", allkv + [("posT", 0), ("posT", 1)], [("n_BT", ii)], out=BT[:, ii, :],
                                   in0=kvcT[:, i:i + 2017:16], scalar1=self.posT[:, i:i + 1], scalar2=None, op0=ALU.add)
                        for kv in range(2):
                            for jc in range(2):
                                pb = 2 * kv + jc
                                for ii in range(16):
                                    i = 16 * ih + ii
                                    self.MM([("n_W1", kv), ("n_BT", ii)], [("ps", pb)], out=self.ps[pb][:, 0:127],
                                            lhsT=W1[64 * kv:64 * kv + 64, ii, 128 * jc:128 * (jc + 1)], rhs=BT[64 * kv:64 * kv + 64, ii, :],
                                            start=(i == 0), stop=(i == 31))
                    for kv in range(2):
                        for jc in range(2):
                            pb = 2 * kv + jc
                            self.A([("ps", pb)], ["n_ex"], out=ex[:], in_=self.ps[pb][:, 0:127], func=AF.Exp, scale=-1.0)
                            self.V("tensor_scalar", ["n_ex"], ["n_ex"], out=ex[:], in0=ex[:], scalar1=1.0, scalar2=None, op0=ALU.add)
                            self.V("reciprocal", ["n_ex"], ["n_ex2"], out=ex[:], in_=ex[:])
                            self.V("tensor_tensor", [("ps", pb), "n_ex2"], [("n_hdT", kv, jc)], out=hdT[:, kv, jc, :], in0=self.ps[pb][:, 0:127],
                                   in1=ex[:], op=ALU.mult)
                    for kv in range(2):
                        pb = 4 + kv
                        for jc in range(2):
                            self.MM([("n_hdT", kv, jc), ("n_w2c", kv)], [("ps", pb)], out=self.ps[pb][0:127, 0:64], lhsT=hdT[:, kv, jc, :],
                                    rhs=w2c[:, kv, jc, :], start=(jc == 0), stop=(jc == 1))
                    self.A([("ps", 4)], ["n_junk", "n_css"], out=junk[0:127, :], in_=self.ps[4][0:127, 0:64], func=AF.Square,
                           accum_out=st[0:127, 38:39])
                    self.rstd(st[0:127, 38:39], "n_css", st[0:127, 39:40], "n_cr", st[0:127, 40:41], "n_crs", 64)
                    self.V("memset", [], ["n_kcd"], ap=kcd[:], constant=0.0)
                    self.V("tensor_scalar", [("ps", 4), "n_crs", "n_kcd"], ["n_kcd"], out=kcd[0:127, :, :],
                           in0=self.ps[4][0:127, 0:64].unsqueeze(1).to_broadcast([127, 2, 64]), scalar1=st[0:127, 40:41], scalar2=None,
                           op0=ALU.mult)
                    pv6 = self.ps[6][:].bitcast(BF16)
                    self.TR(["n_kcd", "identb"], [("ps", 6)], out=pv6[:, 0:128], in_=kcd[:].rearrange("p a d -> p (a d)"),
                            identity=self.identb[:])
                    self.V("tensor_scalar", [("ps", 6), ("gcol", 3, 0), ("gcol", 3, 1)], ["n_kcT"], out=kcT[:], in0=pv6[:, 0:128],
                           scalar1=self.gcol[:, 3:4], scalar2=None, op0=ALU.mult)
                    self.V("memset", [], ["n_vc"], ap=vcs[:], constant=0.0)
                    self.A([("ps", 5), "n_vc"], ["n_vc"], out=vcs[0:127, :], in_=self.ps[5][0:127, 0:64], func=AF.Copy)
            self.barrier()
            if self.stop_after in ((l, "nsa1"), (l, "nsa2")):
                return
            Ycmb2 = [sb("n_Y%d" % i, [128, 4, 256], F32) for i in range(2)]
            nselT = [sb("n_nselT%d" % i, [128, 512], BF16) for i in range(2)]
            pc = sb("n_pc", [128, 4, 128], F32)
            dtc = sb("n_dtc", [128, 4, 128], F32)
            pcb = sb("n_pcb", [128, 4, 128], BF16)
            pcT = sb("n_pcT", [128, 4, 128], BF16)
            P4 = sb("n_P4", [128, 128], F32)
            sc = sb("n_sc", [128, 4, 32], F32)
            m8 = sb("n_m8", [128, 16], F32)
            nsel = sb("n_nsel", [128, 96], BF16)
            self.V("memset", [], ["n_nsel"], ap=nsel[:], constant=0.0)
            coef = sb("n_coef", [128, 8], F32)
            self.V("memset", [], ["n_pc"], ap=pc[:], constant=0.0)

            def cmp_stage(qb):
                Ycmb = Ycmb2[qb % 2]
                for j in range(4):
                    t = 4 * qb + j
                    for h in range(4):
                        p0 = 64 * (h % 2)
                        pbk = 6 + (h % 2)
                        self.MM([("n_qT", t), "n_kcT"], [("ps", pbk)], out=self.ps[pbk][:, 128 * (h // 2):128 * (h // 2) + 128],
                                lhsT=qT[p0:p0 + 64, h // 2, 128 * t:128 * (t + 1)], rhs=kcT[p0:p0 + 64, :], start=True, stop=True)
                    yield
                    off = 120 - 8 * t
                    for par in range(2):
                        sv_ = self.ps[6 + par][:, 0:256].rearrange("p (h n) -> p h n", h=2)
                        self.V("scalar_tensor_tensor", [("ps", 6 + par), "n_tbl"], [("n_dtc", par)], out=dtc[:, par:4:2, :], in0=sv_, scalar=0.125,
                               in1=TcFull[:, par:4:2, off:off + 128], op0=ALU.mult, op1=ALU.add)
                    yield
                    self.A([("n_dtc", 0), ("n_dtc", 1), "n_pc"], ["n_pc"], out=pc[:], in_=dtc[:], func=AF.Exp)
                    yield
                    self.V("tensor_reduce", ["n_pc"], ["n_crs4"], out=st[:, 44:48], in_=pc[:], axis=AX.X, op=ALU.add)
                    self.V("tensor_scalar", ["n_crs4"], ["n_crs4b"], out=st[:, 48:52], in0=st[:, 44:48], scalar1=1e-30, scalar2=None,
                           op0=ALU.max)
                    self.V("reciprocal", ["n_crs4b"], ["n_cri"], out=st[:, 52:56], in_=st[:, 48:52])
                    yield
                    self.V("tensor_tensor", ["n_pc", "n_cri"], ["n_pc"], out=pc[:], in0=pc[:],
                           in1=st[:, 52:56].unsqueeze(2).to_broadcast([128, 4, 128]), op=ALU.mult)
                    yield
                    self.V("tensor_copy", ["n_pc"], ["n_pcb"], out=pcb[:], in_=pc[:])
                    yield
                    pv7 = self.ps[2][:].bitcast(BF16)
                    for h in range(4):
                        self.TR(["n_pcb", "identb"], [("ps", 2)], out=pv7[:, 128 * h:128 * (h + 1)], in_=pcb[:, h, :],
                                identity=self.identb[:])
                    yield
                    self.A([("ps", 2)], ["n_pcT"], out=pcT[:], in_=pv7[:, 0:512].rearrange("p (h n) -> p h n", h=4), func=AF.Copy)
                    yield
                    for h in range(4):
                        self.MM(["n_pcT", "n_vc"], [("ps", 3)], out=self.ps[3][:, 64 * h:64 * (h + 1)], lhsT=pcT[:, h, :],
                                rhs=vcs[:, :], start=True, stop=True)
                    yield
                    for h in range(4):
                        self.V("tensor_scalar", [("ps", 3), ("n_g", t)], [("n_Y", qb % 2, j, h)], out=Ycmb[:, j, 64 * h:64 * (h + 1)],
                               in0=self.ps[3][:, 64 * h:64 * (h + 1)], scalar1=gates[:, t, 3 * h:3 * h + 1], scalar2=None, op0=ALU.mult)
                    yield
                    self.V("tensor_reduce", ["n_pc"], ["n_P4"], out=P4[:], in_=pc[:].rearrange("p h n -> p n h"), axis=AX.X, op=ALU.add)
                    P4v = P4[:].rearrange("p (j r) -> p j r", r=4)
                    yield
                    self.V("tensor_reduce", ["n_P4"], [("n_sc", 0)], out=sc[:, 0, :], in_=P4v[:, :, 0:3], axis=AX.X, op=ALU.add)
                    self.V("scalar_tensor_tensor", ["n_P4", ("n_sc", 0)], [("n_sc", 1)], out=sc[:, 1, :], in0=P4v[:, :, 3], scalar=0.5,
                           in1=sc[:, 0, :], op0=ALU.mult, op1=ALU.add)
                    yield
                    self.V("scalar_tensor_tensor", ["n_P4", ("n_sc", 1)], [("n_sc", 1)], out=sc[:, 1, 1:32], in0=P4v[:, 0:31, 3], scalar=0.5,
                           in1=sc[:, 1, 1:32], op0=ALU.mult, op1=ALU.add)
                    yield
                    self.V("tensor_tensor", [("n_sc", 1), "n_vm"], [("n_sc", 2)], out=sc[:, 2, :], in0=sc[:, 1, :], in1=vm[:, t, :],
                           op=ALU.mult)
                    self.V("tensor_tensor", [("n_sc", 2), "n_ad"], [("n_sc", 2)], out=sc[:, 2, :], in0=sc[:, 2, :], in1=ad[:, t, :],
                           op=ALU.add)
                    yield
                    self.V("max", [("n_sc", 2)], ["n_m8a"], out=m8[:, 0:8], in_=sc[:, 2, :])
                    yield
                    self.V("match_replace", [("n_sc", 2), "n_m8a"], [("n_sc", 3)], out=sc[:, 3, :], in_to_replace=m8[:, 0:8],
                           in_values=sc[:, 2, :], imm_value=-2.0 * BIG)
                    yield
                    self.V("max", [("n_sc", 3)], ["n_m8b"], out=m8[:, 8:16], in_=sc[:, 3, :])
                    yield
                    for dup in range(2):
                        self.V("tensor_scalar", [("n_sc", 2), "n_m8b", "n_nsel"], ["n_nsel"], out=nsel[:, 64 * dup:64 * dup + 32], in0=sc[:, 2, :],
                               scalar1=m8[:, 15:16], scalar2=NEG, op0=ALU.is_lt, op1=ALU.mult)
                    yield
                    self.TR(["n_nsel", "identb"], [("ps", 2)], out=pv7[0:96, 512:640], in_=nsel[:], identity=self.identb[:])
                    yield
                    self.V("tensor_copy", [("ps", 2)], [("n_nselT", qb % 2, j)], out=nselT[qb % 2][0:96, 128 * j:128 * (j + 1)],
                           in_=pv7[0:96, 512:640])

            items = []
            g = 0
            for qb in range(4):
                for br in ("slc", "win"):
                    for h in range(4):
                        k0 = 0 if br == "slc" else max(0, 4 * qb - 4)
                        nk = 4 * qb + 4
                        for kt in range(k0, nk):
                            lo = max(kt, 4 * qb)
                            hi = 4 * qb + 3 if br == "slc" else min(kt + 4, 4 * qb + 3)
                            tiles = []
                            start, stop = {}, {}
                            for qt in range(lo, hi + 1):
                                j = qt - 4 * qb
                                if qt == kt:
                                    kind = (TD[:, h, :], ["n_tbl"])
                                elif qt == kt + 1:
                                    kind = (TO[:, h, :], ["n_tbl"])
                                elif br == "win" and qt == kt + 4:
                                    kind = (TW[:, h, :], ["n_tbl"])
                                else:
                                    kind = None
                                tiles.append((j, kind))
                                start[j] = (kt == (0 if br == "slc" else max(0, qt - 4)))
                                stop[j] = (kt == qt)
                            it = dict(qb=qb, h=h, kt=kt, g=g, br=br, last=(kt == nk - 1), gfirst=(kt == k0), tiles=tiles, start=start,
                                      stop=stop)
                            items.append(it)
                        g += 1

            for qb in range(4):
                qi = [it for it in items if it["qb"] == qb]
                qi[0]["qb_first"] = True
                qi[-1]["qb_last"] = True
                qi[0]["ksteps"] = -(-84 // max(1, len(qi) - 8))
            gstate = {"gen": None, "k": 1}
            for _ in cmp_stage(0):
                pass

            def advance(it):
                if it.get("qb_first"):
                    gstate["gen"] = cmp_stage(it["qb"] + 1) if it["qb"] < 3 else None
                    gstate["k"] = it["ksteps"]
                g_ = gstate["gen"]
                if g_ is None:
                    return
                n = 10 ** 6 if it.get("qb_last") else gstate["k"]
                for _ in range(n):
                    try:
                        next(g_)
                    except StopIteration:
                        gstate["gen"] = None
                        break

            def score_fn(it, zb, c0, c1):
                qb, h, kt = it["qb"], it["h"], it["kt"]
                advance(it)
                p0 = 64 * (h % 2)
                qres = [("n_qT", t) for t in range(4 * qb, 4 * qb + 4)]
                if it["br"] == "slc":
                    self.MM(qres + [("n_ksT", kt)], [("ps", zb)], out=self.ps[zb][:, c0:c1], lhsT=ksT[p0:p0 + 64, 128 * kt:128 * (kt + 1)],
                            rhs=qT[p0:p0 + 64, h // 2, 512 * qb + c0:512 * qb + c1], start=True, stop=False)
                    self.MM([("n_E", 0), ("n_E", 1)] + [("n_nselT", qb % 2, j) for j in range(4)], [("ps", zb)], out=self.ps[zb][:, c0:c1],
                            lhsT=E[p0:p0 + 32, 128 * kt:128 * (kt + 1)], rhs=nselT[qb % 2][p0:p0 + 32, c0:c1], start=False, stop=True)
                else:
                    self.MM(qres + [("n_kwT", kt)], [("ps", zb)], out=self.ps[zb][:, c0:c1], lhsT=kwT[p0:p0 + 64, 128 * kt:128 * (kt + 1)],
                            rhs=qT[p0:p0 + 64, h // 2, 512 * qb + c0:512 * qb + c1], start=True, stop=True)

            def v_fn(it):
                if it["br"] == "slc":
                    return vsa[:, it["kt"], :], [("n_vs", it["kt"])], 65
                return vwa[:, it["kt"], :], [("n_vw", it["kt"])], 65

            def finish(it, acc):
                qb, h = it["qb"], it["h"]
                Ycmb = Ycmb2[qb % 2]
                gi = 3 * h + (1 if it["br"] == "slc" else 2)
                av = self.ps[acc][:, 0:260].rearrange("p (j d) -> p j d", j=4)
                self.V("tensor_scalar", [("ps", acc)], ["n_rsum"], out=st[:, 60:64], in0=av[:, :, 64], scalar1=1e-30, scalar2=None,
                       op0=ALU.max)
                self.V("reciprocal", ["n_rsum"], ["n_rinv"], out=st[:, 56:60], in_=st[:, 60:64])
                self.V("tensor_tensor", ["n_rinv"] + [("n_g", t) for t in range(4 * qb, 4 * qb + 4)], ["n_coef"], out=coef[:, 0:4],
                       in0=st[:, 56:60], in1=gates[:, 4 * qb:4 * qb + 4, gi], op=ALU.mult)
                for j in range(4):
                    self.V("scalar_tensor_tensor", [("ps", acc), "n_coef", ("n_Y", qb % 2, j, h)], [("n_Y", qb % 2, j, h)], out=Ycmb[:, j, 64 * h:64 * (h + 1)],
                           in0=av[:, j, 0:64], scalar=coef[:, j:j + 1], in1=Ycmb[:, j, 64 * h:64 * (h + 1)], op0=ALU.mult, op1=ALU.add)
                if it["br"] == "win" and h == 3:
                    for j in range(4):
                        self.group_norm(Ycmb[:, j, :], [("n_Y", qb % 2, j, hh) for hh in range(4)], 1, 4 * qb + j, j % 2)

            if self.stop_after == (l, "nsa3"):
                return
            self.attn_core(abufs, "n", 64, items, score_fn, None, 0.125, v_fn, finish, cbias=lambda it: self.c31[:, it["h"]:it["h"] + 1])
            self.dump("yn_nsa", self.Yn[:, :, 256:512], [128, NT, 256], [("Yn", t, 1) for t in range(NT)])

    def out_proj(self, l):
        dr = self.dram
        with ExitStack() as es:
            wo = self.sb("o_w", [128, 8, D], BF16, es)
            yT = [self.sb("o_yT%d" % i, [128, 8, 128], BF16, es) for i in range(2)]
            src = dr["w_out"][l].rearrange("(c p) n -> p c n", p=128)
            for q in range(4):
                self.DMA("pool", [], [("o_w", q)], "ow%d" % q, out=wo[:, 2 * q:2 * q + 2, :], in_=src[:, 2 * q:2 * q + 2, :])
            ong = self.sb("o_g", [128, 8], F32, es)
            onl = dr["out_norm_w"][l]
            self.DMA("sp", [], ["o_g"], "p3", out=ong[:], in_=bass.AP(tensor=onl.tensor, offset=onl.offset, ap=[[1, 128], [128, 8]]),
                     allow_slow_non_contiguous=True)
            for c in range(8):
                self.I("pool" if c % 2 else "dve", "tensor_scalar", [("o_w", c // 2), "o_g"], [("o_w", c // 2)], out=wo[:, c, :], in0=wo[:, c, :],
                       scalar1=ong[:, c:c + 1], scalar2=None, op0=ALU.mult)
            wres = [("o_w", q) for q in range(4)]
            for t in range(NT):
                b = t % 2
                pb = 6 + b
                pv = self.ps[pb][:].bitcast(BF16)
                for c in range(8):
                    self.TR([("Yn", t, c // 2), "identb"], [("ps", pb)], out=pv[:, 128 * c:128 * (c + 1)],
                            in_=self.Yn[:, t, 128 * c:128 * (c + 1)], identity=self.identb[:])
                self.A([("ps", pb)], [("o_yT", b)], out=yT[b][:], in_=pv.rearrange("p (c n) -> p c n", c=8), func=AF.Copy)
                for hf in range(2):
                    ob = 2 * b + hf
                    for c in range(8):
                        self.MM([("o_yT", b)] + wres, [("ps", ob)], out=self.ps[ob][:, :], lhsT=yT[b][:, c, :],
                                rhs=wo[:, c, 512 * hf:512 * (hf + 1)], start=(c == 0), stop=(c == 7))
                    self.V("tensor_tensor", [("ps", ob), ("X", t)], [("X", t)], out=self.X[:, t, 512 * hf:512 * (hf + 1)],
                           in0=self.ps[ob][:, :], in1=self.X[:, t, 512 * hf:512 * (hf + 1)], op=ALU.add)
            self.dump("xmid", self.X[:], [128, NT, D], [("X", t) for t in range(NT)])

    def ffn(self, l):
        dr = self.dram
        hT = self.hT
        uT = self.Yn
        with ExitStack() as es:
            w2h = self.sb("f_w2", [128, 16, D], BF16, es)
            w1c = [self.sb("f_w1_%d" % i, [128, 8, 128], BF16, es) for i in range(3)]
            self.f_r = [self.sb("f_r%d" % i, [128, 512], F32, es) for i in range(4)]
            w1src = dr["ffn_w1"][l].rearrange("(c p) f -> p c f", p=128)
            w2src = dr["ffn_w2"][l].rearrange("(c p) n -> p c n", p=128)
            k = 0
            for fh in range(2):
                for q in range(4):
                    self.DMA("pool", [], [("f_w2", q)], "fw2%d" % q, out=w2h[:, 4 * q:4 * q + 4, :],
                             in_=w2src[:, 16 * fh + 4 * q:16 * fh + 4 * q + 4, :])
                for th in range(2):
                    for fc in range(16):
                        wb = k % 3
                        k += 1
                        f0 = (16 * fh + fc) * 128
                        self.DMA("pool", [], [("f_w1", wb)], "fw1%d" % wb, out=w1c[wb][:], in_=w1src[:, :, f0:f0 + 128])
                        for tb2 in range(2):
                            tb = 2 * th + tb2
                            pb = (2 * fc + tb2) % 4
                            for dc in range(8):
                                self.MM(self.hT_res(tb) + [("f_w1", wb)], [("ps", pb)], out=self.ps[pb][:, :], lhsT=w1c[wb][:, dc, :],
                                        rhs=hT[:, dc, 512 * tb:512 * (tb + 1)], start=(dc == 0), stop=(dc == 7))
                            dst = uT[:, fc, 512 * tb2:512 * (tb2 + 1)]
                            self.A([("ps", pb)], [("f_r", pb)], out=self.f_r[pb][:], in_=self.ps[pb][:, :], func=AF.Relu)
                            self.V("tensor_tensor", [("f_r", pb)], [("f_u", fc, tb2)], out=dst, in0=self.f_r[pb][:], in1=self.f_r[pb][:],
                                   op=ALU.mult)
                    for tt in range(8):
                        t = 8 * th + tt
                        for hf in range(2):
                            ob = 4 + (2 * tt + hf) % 4
                            for fc in range(16):
                                self.MM([("f_u", fc, tt // 4), ("f_w2", fc // 4)], [("ps", ob)], out=self.ps[ob][:, :],
                                        lhsT=uT[:, fc, 128 * tt:128 * (tt + 1)], rhs=w2h[:, fc, 512 * hf:512 * (hf + 1)],
                                        start=(fc == 0), stop=(fc == 15))
                            self.V("tensor_tensor", [("ps", ob), ("X", t)], [("X", t)], out=self.X[:, t, 512 * hf:512 * (hf + 1)],
                                   in0=self.ps[ob][:, :], in1=self.X[:, t, 512 * hf:512 * (hf + 1)], op=ALU.add)


_CACHE = {}


def run(inputs, n_cores=8, stop_after=None, dbg=(), trace=False):
    key = (stop_after, tuple(dbg))
    b = Builder(stop_after=stop_after, dbg=dbg)
    nc = b.build()
    consts = make_consts()
    shared = {k: np.ascontiguousarray(np.asarray(inputs[k], dtype=np.float32)) for k in WEIGHT_SHAPES}
    if stop_after is not None:
        shared["ffn_w1"] = np.zeros((NL, 128, 128), np.float32)
        shared["ffn_w2"] = np.zeros((NL, 128, 128), np.float32)
    shared.update(consts)
    x = np.asarray(inputs["x"], dtype=np.float32)
    in_maps = []
    for i in range(n_cores):
        m = dict(shared)
        m["x"] = np.ascontiguousarray(x[i])
        in_maps.append(m)
    res = run_bass_kernel_spmd(nc, in_maps, core_ids=list(range(n_cores)), trace=trace)
    return res, b


def kernel(**inputs):
    res, b = run(inputs, n_cores=8)
    out = np.stack([np.asarray(r["out"], dtype=np.float32) for r in res.results], axis=0)
    return out
```
